# Optimizing a Trainium2 kernel written in Bass

```python
import math
import jax, jax.numpy as jnp
from jax import lax
import numpy as np

D_MODEL = 1024
BATCH = 32
SEQ = 2048
DEPTH = 1

PLE_DIM = 256
N_DIFF_HEADS = 8
DIFF_HEAD_DIM = 64
DIFF_V_DIM = 2 * DIFF_HEAD_DIM
N_RET_HEADS = 4
RET_KEY_DIM = 128
RET_VAL_DIM = 2 * RET_KEY_DIM
D_FF = 4 * D_MODEL
Q_BLOCK = 128
RET_CHUNK = 128
EPS = 1e-6

DIFF_QK_W = N_DIFF_HEADS * 2 * DIFF_HEAD_DIM
DIFF_V_W = N_DIFF_HEADS * DIFF_V_DIM
RET_QK_W = N_RET_HEADS * RET_KEY_DIM
RET_V_W = N_RET_HEADS * RET_VAL_DIM
IN_SPLITS = (DIFF_QK_W, DIFF_QK_W, DIFF_V_W,
             RET_QK_W, RET_QK_W, RET_V_W, RET_V_W,
             D_MODEL, D_MODEL)
D_IN = sum(IN_SPLITS)

kernel_name = "hybrid_diffattn_retention_gated_block"


def rms_norm(x, g):
    xf = x.astype(jnp.float32)
    y = xf * lax.rsqrt(jnp.mean(xf * xf, axis=-1, keepdims=True) + EPS)
    return (y * g.astype(jnp.float32)).astype(x.dtype)


def alibi_slopes(n_heads):
    return 2.0 ** (-8.0 * jnp.arange(1, n_heads + 1, dtype=jnp.float32) / n_heads)


def diff_attention(q, k, v, lam):
    B, S, H, _, d = q.shape
    e = v.shape[-1]
    n_blocks = S // Q_BLOCK
    scale = d ** -0.5
    kpos = jnp.arange(S)
    slopes = alibi_slopes(H)[:, None, None, None]

    def block(i):
        start = i * Q_BLOCK
        qb = lax.dynamic_slice_in_dim(q, start, Q_BLOCK, axis=1)
        s = jnp.einsum('bqhcd,bkhcd->bhcqk', qb, k).astype(jnp.float32) * scale
        qpos = start + jnp.arange(Q_BLOCK)
        dist = (qpos[:, None] - kpos[None, :]).astype(jnp.float32)
        logits = jnp.where(dist >= 0, s - slopes * dist, -jnp.inf)
        probs = jax.nn.softmax(logits, axis=-1)
        a = probs[:, :, 0] - lam * probs[:, :, 1]
        return jnp.einsum('bhqk,bkhe->bqhe', a.astype(v.dtype), v)

    out = lax.map(block, jnp.arange(n_blocks))
    return out.transpose(1, 0, 2, 3, 4).reshape(B, S, H, e)


def retention(q, k, v):
    B, S, H, dk = q.shape
    dv = v.shape[-1]
    C = RET_CHUNK
    n_chunks = S // C
    dt = q.dtype
    log_gamma = jnp.log(1.0 - 2.0 ** (-5.0 - jnp.arange(H, dtype=jnp.float32)))
    pos = jnp.arange(C, dtype=jnp.float32)
    rel = pos[:, None] - pos[None, :]
    decay_intra = jnp.where(rel >= 0, jnp.exp(log_gamma[:, None, None] * rel), 0.0).astype(dt)
    q_decay = jnp.exp(log_gamma[None, :] * (pos[:, None] + 1.0)).astype(dt)
    k_decay = jnp.exp(log_gamma[:, None] * (C - 1.0 - pos[None, :])).astype(dt)
    chunk_decay = jnp.exp(log_gamma * C).astype(dt)

    k = k * (dk ** -0.5)
    to_chunks = lambda t: t.reshape(B, n_chunks, C, H, t.shape[-1]).transpose(1, 0, 2, 3, 4)
    qc, kc, vc = to_chunks(q), to_chunks(k), to_chunks(v)

    def step(R, inp):
        qi, ki, vi = inp
        s = jnp.einsum('bnhk,bmhk->bhnm', qi, ki) * decay_intra
        intra = jnp.einsum('bhnm,bmhv->bnhv', s, vi)
        cross = jnp.einsum('bnhk,bhkv->bnhv', qi, R) * q_decay[None, :, :, None]
        R_new = R * chunk_decay[None, :, None, None] + jnp.einsum('bmhk,bmhv,hm->bhkv', ki, vi, k_decay)
        return R_new, intra + cross

    R0 = jnp.zeros((B, H, dk, dv), dt)
    _, out = lax.scan(step, R0, (qc, kc, vc))
    return out.transpose(1, 0, 2, 3, 4).reshape(B, S, H, dv)


def setup_inputs(seed: int = 0) -> dict:
    key = jax.random.key(seed)
    ks = jax.random.split(key, 24)
    f32 = jnp.float32
    nrm = lambda k, shape, s: (jax.random.normal(k, shape, f32) * s)
    gain = lambda k, shape: 1.0 + 0.02 * jax.random.normal(k, shape, f32)
    L = DEPTH
    return {
        "x": nrm(ks[0], (BATCH, SEQ, D_MODEL), 1.0),
        "p": nrm(ks[1], (DEPTH, BATCH, SEQ, PLE_DIM), 1.0),
        "g_mix": gain(ks[2], (L, D_MODEL)),
        "w_in": nrm(ks[3], (L, D_MODEL, D_IN), D_MODEL ** -0.5),
        "lam_q1": nrm(ks[4], (L, DIFF_HEAD_DIM), 0.1),
        "lam_k1": nrm(ks[5], (L, DIFF_HEAD_DIM), 0.1),
        "lam_q2": nrm(ks[6], (L, DIFF_HEAD_DIM), 0.1),
        "lam_k2": nrm(ks[7], (L, DIFF_HEAD_DIM), 0.1),
        "g_diff_sub": gain(ks[8], (L, DIFF_V_DIM)),
        "g_ret_sub": gain(ks[9], (L, N_RET_HEADS, RET_VAL_DIM)),
        "w_branch_diff": nrm(ks[10], (L, DIFF_V_W, D_MODEL), DIFF_V_W ** -0.5),
        "w_branch_ret": nrm(ks[11], (L, RET_V_W, D_MODEL), RET_V_W ** -0.5),
        "w_out": nrm(ks[12], (L, D_MODEL, D_MODEL), D_MODEL ** -0.5),
        "g_mlp": gain(ks[13], (L, D_MODEL)),
        "w_ff1": nrm(ks[14], (L, D_MODEL, D_FF), D_MODEL ** -0.5),
        "w_ff2": nrm(ks[15], (L, D_FF, D_MODEL), D_FF ** -0.5),
        "g_ple": gain(ks[16], (L, D_MODEL)),
        "w_ple_gate": nrm(ks[17], (L, D_MODEL, D_MODEL), D_MODEL ** -0.5),
        "w_ple": nrm(ks[18], (L, PLE_DIM, D_MODEL), PLE_DIM ** -0.5),
        "g_final": gain(ks[19], (D_MODEL,)),
    }


def reference(x, p, g_mix, w_in, lam_q1, lam_k1, lam_q2, lam_k2, g_diff_sub, g_ret_sub,
              w_branch_diff, w_branch_ret, w_out, g_mlp, w_ff1, w_ff2, g_ple, w_ple_gate,
              w_ple, g_final):
    B, S, _ = x.shape
    split_idx = list(np.cumsum(IN_SPLITS)[:-1])
    for i in range(DEPTH):
        h = rms_norm(x, g_mix[i])
        proj = h @ w_in[i]
        qa, ka, va, qr, kr, vr, gr, gate_a, gate_r = jnp.split(proj, split_idx, axis=-1)

        lam_init = 0.8 - 0.6 * math.exp(-0.3 * i)
        lam = (jnp.exp(jnp.sum(lam_q1[i] * lam_k1[i]).astype(jnp.float32))
               - jnp.exp(jnp.sum(lam_q2[i] * lam_k2[i]).astype(jnp.float32)) + lam_init)
        qa = qa.reshape(B, S, N_DIFF_HEADS, 2, DIFF_HEAD_DIM)
        ka = ka.reshape(B, S, N_DIFF_HEADS, 2, DIFF_HEAD_DIM)
        va = va.reshape(B, S, N_DIFF_HEADS, DIFF_V_DIM)
        ya = diff_attention(qa, ka, va, lam)
        ya = (rms_norm(ya, g_diff_sub[i]) * (1.0 - lam_init)).reshape(B, S, DIFF_V_W)

        qr = qr.reshape(B, S, N_RET_HEADS, RET_KEY_DIM)
        kr = kr.reshape(B, S, N_RET_HEADS, RET_KEY_DIM)
        vr = vr.reshape(B, S, N_RET_HEADS, RET_VAL_DIM)
        yr = rms_norm(retention(qr, kr, vr), g_ret_sub[i]).reshape(B, S, RET_V_W)
        yr = jax.nn.silu(gr) * yr

        mixed = (jax.nn.sigmoid(gate_a) * (ya @ w_branch_diff[i])
                 + jax.nn.sigmoid(gate_r) * (yr @ w_branch_ret[i]))
        x = x + mixed @ w_out[i]

        h2 = rms_norm(x, g_mlp[i])
        x = x + jnp.square(jax.nn.relu(h2 @ w_ff1[i])) @ w_ff2[i]

        gate_p = jax.nn.sigmoid(rms_norm(x, g_ple[i]) @ w_ple_gate[i])
        x = x + gate_p * (p[i] @ w_ple[i])
    return rms_norm(x, g_final)
```

```python
import contextlib
import math
import numpy as np
import concourse.bass as bass
import concourse.mybir as mybir
from concourse.bass_utils import run_bass_kernel_spmd

F32 = mybir.dt.float32
BF16 = mybir.dt.bfloat16
AF = mybir.ActivationFunctionType
ALU = mybir.AluOpType
AX = mybir.AxisListType


def _norm_idx(idx, shape):
    if not isinstance(idx, tuple):
        idx = (idx,)
    box = []
    for d, n in enumerate(shape):
        if d < len(idx):
            i = idx[d]
            if isinstance(i, slice):
                lo = 0 if i.start is None else i.start
                hi = n if i.stop is None else i.stop
            else:
                lo, hi = int(i), int(i) + 1
        else:
            lo, hi = 0, n
        assert 0 <= lo < hi <= n, (idx, shape)
        box.append((lo, hi))
    return tuple(box)


class View:
    __slots__ = ("buf", "ap", "box")

    def __init__(self, buf, ap, box):
        self.buf, self.ap, self.box = buf, ap, box

    def with_ap(self, fn):
        return View(self.buf, fn(self.ap), self.box)

    def cols(self, lo, hi):
        b0 = self.box[-1][0]
        return View(self.buf, self.ap[:, lo:hi], self.box[:-1] + ((b0 + lo, b0 + hi),))


class Buf:
    def __init__(self, prog, name, handle, shape, tracked=True, whole=False):
        self.prog, self.name, self.handle, self.shape = prog, name, handle, tuple(shape)
        self.tracked = tracked
        self.whole = whole
        self.group = None
        self.hist = []

    def __getitem__(self, idx):
        box = _norm_idx(idx, self.shape)
        if self.whole:
            box = tuple((0, n) for n in self.shape)
        return View(self, self.handle[idx], box)

    def full(self):
        return self[tuple(slice(None) for _ in self.shape)]


def _overlap(a, b):
    for (l0, h0), (l1, h1) in zip(a, b):
        if h0 <= l1 or h1 <= l0:
            return False
    return True


def _contains(outer, inner):
    for (l0, h0), (l1, h1) in zip(outer, inner):
        if l1 < l0 or h1 > h0:
            return False
    return True


class Instr:
    __slots__ = ("eng", "fn", "clock", "pos", "waits", "signal", "is_dma", "snap")


ENGS = ("pe", "act", "dve", "pool", "sp")
DMA_SLOTS = {"sp": 8, "act": 4, "pool": 8}


class Prog:
    def __init__(self, nc):
        self.nc = nc
        self.stack = contextlib.ExitStack()
        self.instrs = {e: [] for e in ENGS}
        self.known = {e: {} for e in ENGS}
        self.clock_pos = {}
        self.clock_instrs = {}
        self.dma_n = {e: 0 for e in DMA_SLOTS}
        self.n_total = 0

    def sbuf(self, name, shape, dtype):
        h = self.stack.enter_context(self.nc.sbuf_tensor(name, list(shape), dtype))
        return Buf(self, name, h, shape)

    def psum(self, name, shape, dtype=F32):
        h = self.stack.enter_context(self.nc.psum_tensor(name, list(shape), dtype))
        return Buf(self, name, h, shape, whole=True)

    def dram(self, name, shape, dtype, kind="Internal", tracked=True):
        h = self.nc.dram_tensor(name, list(shape), dtype, kind=kind)
        return Buf(self, name, h, shape, tracked=tracked)

    def alias(self, name, base, new_handle, shape, whole=False):
        b = Buf(self, name, new_handle, shape, whole=whole)
        if base.group is None:
            base.group = [base]
        base.group.append(b)
        b.group = base.group
        return b

    def _deps_for(self, view, is_write, deps, eng):
        buf = view.buf
        if not buf.tracked:
            return
        bufs = buf.group if buf.group is not None else (buf,)
        for b in bufs:
            same = b is buf
            for ebox, ewrite, eclock, epos in b.hist:
                if not (is_write or ewrite):
                    continue
                if same and not _overlap(ebox, view.box):
                    continue
                if eclock == "pe" and eng == "pe":
                    continue
                if deps.get(eclock, 0) < epos:
                    deps[eclock] = epos

    def _record(self, view, is_write, clock, pos, in_order):
        buf = view.buf
        if not buf.tracked:
            return
        box = view.box
        h = buf.hist
        if is_write:
            h[:] = [e for e in h if not _contains(box, e[0])]
            if buf.group is not None and buf.whole:
                for b in buf.group:
                    if b is not buf:
                        b.hist[:] = []
        elif in_order:
            h[:] = [e for e in h if not (e[2] == clock and not e[1] and _contains(box, e[0]))]
        h.append((box, is_write, clock, pos))

    def op(self, eng, fn, reads=(), writes=(), dma=False):
        ins = Instr()
        ins.eng, ins.fn, ins.is_dma, ins.signal = eng, fn, dma, dma
        if dma:
            n = self.dma_n[eng]
            self.dma_n[eng] = n + 1
            clock = ("dma", eng, n % DMA_SLOTS[eng])
        else:
            clock = eng
        pos = self.clock_pos.get(clock, 0) + 1
        self.clock_pos[clock] = pos
        self.clock_instrs.setdefault(clock, []).append(ins)
        ins.clock, ins.pos = clock, pos
        deps = {}
        if dma and pos > 1:
            deps[clock] = pos - 1
        for v in reads:
            self._deps_for(v, False, deps, eng)
        for v in writes:
            self._deps_for(v, True, deps, eng)
        known = self.known[eng]
        waits = []
        for ck, p in deps.items():
            if known.get(ck, 0) >= p:
                continue
            waits.append((ck, p))
            j = self.clock_instrs[ck][p - 1]
            j.signal = True
            for k2, p2 in j.snap.items():
                if known.get(k2, 0) < p2:
                    known[k2] = p2
            known[ck] = p
        ins.waits = waits
        ins.snap = dict(known)
        if fn is not None:
            for v in reads:
                self._record(v, False, clock, pos, not dma)
            for v in writes:
                self._record(v, True, clock, pos, not dma)
        self.instrs[eng].append(ins)
        self.n_total += 1
        return ins

    def dma(self, queue, out, in_, **kw):
        def fn(e):
            return e.dma_start(out=out.ap, in_=in_.ap, **kw)
        return self.op(queue, fn, reads=[in_], writes=[out], dma=True)

    def barrier(self, eng, views):
        return self.op(eng, None, reads=(), writes=views)

    def emit(self):
        nc = self.nc
        SEM_EPOCH = 1024
        sems = {}
        val = {}
        semof = {}
        for ck, lst in self.clock_instrs.items():
            nm = ck if isinstance(ck, str) else "d_%s_%d" % (ck[1], ck[2])
            step = 1 if isinstance(ck, str) else 16
            c = 0
            for ins in lst:
                if ins.signal:
                    ep, r = divmod(c, SEM_EPOCH // step)
                    c += 1
                    if (ck, ep) not in sems:
                        sems[(ck, ep)] = self.stack.enter_context(nc.semaphore("s_%s_%d" % (nm, ep)))
                    val[(ck, ins.pos)] = (r + 1) * step
                    semof[(ck, ins.pos)] = sems[(ck, ep)]
        engmap = {"pe": "tensor", "act": "scalar", "dve": "vector", "pool": "gpsimd", "sp": "sync"}
        with nc.Block() as block:
            for ename in ENGS:
                lst = self.instrs[ename]

                def body(e, lst=lst):
                    for ins in lst:
                        for ck, p in ins.waits:
                            e.wait_ge(semof[(ck, p)], val[(ck, p)])
                        if ins.fn is None:
                            continue
                        r = ins.fn(e)
                        if ins.signal:
                            r.then_inc(semof[(ins.clock, ins.pos)], 16 if ins.is_dma else 1)

                getattr(block, engmap[ename])(body)

    def close(self):
        self.stack.close()


D = 1024
SEQ = 2048
NSEQ = 4
TOK = NSEQ * SEQ
QB = 512
NQB_SEQ = SEQ // QB
PLE = 256
DFF = 4096
EPS = 1e-6
LAM_INIT = 0.8 - 0.6 * math.exp(-0.3 * 0)
NH = 8
SLOPES = [2.0 ** (-(h + 1)) for h in range(NH)]
SUBW = [128, 256, 512, 512, 512, 512, 512, 512]
GAM = [1.0 - 2.0 ** (-5.0 - h) for h in range(4)]
LNG = [math.log(g) for g in GAM]
CUT = 60.0

WT = {}
_t = 0
for nm, c0 in (("qa", 0), ("ka", 1024), ("va", 2048)):
    for j in range(2):
        WT[(nm, j)] = (_t, "w_in", 0, c0 + 512 * j); _t += 1
WT[("qr", 0)] = (_t, "w_in", 0, 3072); _t += 1
WT[("kr", 0)] = (_t, "w_in", 0, 3584); _t += 1
for nm, c0 in (("vr", 4096), ("gr", 5120), ("ga", 6144), ("gb", 7168)):
    for j in range(2):
        WT[(nm, j)] = (_t, "w_in", 0, c0 + 512 * j); _t += 1
for nm, src in (("wd", "w_branch_diff"), ("wr", "w_branch_ret"), ("wo", "w_out")):
    for j in range(2):
        WT[(nm, j)] = (_t, src, 0, 512 * j); _t += 1
for j in range(8):
    WT[("f1", j)] = (_t, "w_ff1", 0, 512 * j); _t += 1
for n in range(2):
    for g in range(4):
        WT[("f2", n * 4 + g)] = (_t, "w_ff2", 1024 * g, 512 * n); _t += 1
for j in range(2):
    WT[("pg", j)] = (_t, "w_ple_gate", 0, 512 * j); _t += 1
WT[("ple", 0)] = (_t, "w_ple", 0, 0); _t += 1
NWT = _t

QB_SCHED = ([("qa", 0), ("qa", 1), ("ka", 0), ("ka", 1), ("va", 0), ("va", 1), ("qr", 0), ("kr", 0),
             ("vr", 0), ("vr", 1), ("gr", 0), ("gr", 1)]
            + [("ga", 0), ("wd", 0), ("gb", 0), ("wr", 0), ("ga", 1), ("wd", 1), ("gb", 1), ("wr", 1)]
            + [("wo", 0), ("wo", 1)]
            + [("f1", j) for j in range(8)]
            + [("f2", j) for j in range(8)]
            + [("ple", 0), ("pg", 0), ("pg", 1)])


def build_program(n_qb=16, nslots=2, stop=99):
    nc = bass.Bass("TRN2", target_bir_lowering=False)
    P = Prog(nc)
    ext = lambda n, s: P.dram(n, s, F32, kind="ExternalInput", tracked=False)
    x = ext("x", [TOK, D])
    pin = ext("p", [TOK, PLE])
    g_mix = ext("g_mix", [D]); g_mlp = ext("g_mlp", [D]); g_ple = ext("g_ple", [D]); g_final = ext("g_final", [D])
    wsrc = {"w_in": ext("w_in", [D, 8192]), "w_branch_diff": ext("w_branch_diff", [D, D]),
            "w_branch_ret": ext("w_branch_ret", [D, D]), "w_out": ext("w_out", [D, D]),
            "w_ff1": ext("w_ff1", [D, DFF]), "w_ff2": ext("w_ff2", [DFF, D]),
            "w_ple_gate": ext("w_ple_gate", [D, D]), "w_ple": ext("w_ple", [PLE, D])}
    lam_in = [ext(n, [64]) for n in ("lam_q1", "lam_k1", "lam_q2", "lam_k2")]
    g_diff_sub = ext("g_diff_sub", [128])
    g_ret_sub = ext("g_ret_sub", [1024])
    out = P.dram("out", [TOK, D], F32, kind="ExternalOutput")
    wb = P.dram("wb", [NWT, 128, 8, 512], BF16)

    KT = P.sbuf("KT", [128, NH, SEQ], BF16)
    VC = P.sbuf("VC", [128, SEQ // 128, D], BF16)
    wsl = [P.sbuf("wsl%d" % i, [128, 8, 512], BF16) for i in range(nslots)]
    xres = P.sbuf("xres", [128, 4, D], F32)
    hT = P.sbuf("hT", [128, 8, QB], BF16)
    U = P.sbuf("U", [128, 52, QB], BF16)
    QT = lambda h: U[:, h, :]
    tmpf = [P.sbuf("tmpf%d" % i, [128, D], F32) for i in range(3)]
    PT = [P.sbuf("PT%d" % i, [128, QB], BF16) for i in range(4)]
    hb = [P.sbuf("hb%d" % i, [128, D], BF16) for i in range(1)] * 2
    ident = P.sbuf("ident", [128, 128], BF16)
    ones = P.sbuf("ones", [128, 128], BF16)
    iota_p = P.sbuf("iota_p", [128, 1], F32)
    mhalf = P.sbuf("mhalf", [128, 4], F32)
    bias_tab = P.sbuf("bias_tab", [128, NH * 24], F32)
    trimask = P.sbuf("trimask", [128, 128], BF16)
    qdec = P.sbuf("qdec", [128, 4, 128], F32)
    kdec = P.sbuf("kdec", [128, 4, 128], F32)
    cdec = P.sbuf("cdec", [128, 4], F32)
    gcol = P.sbuf("gcol", [128, 3, 8], F32)
    gdcol = P.sbuf("gdcol", [128, 1], F32)
    gret_b = P.sbuf("gret_b", [128, D], F32)
    gfin_b = P.sbuf("gfin_b", [128, D], F32)
    lamt = P.sbuf("lamt", [128, 4, 64], F32)
    lams = P.sbuf("lams", [128, 8], F32)
    stat = P.sbuf("stat", [128, 16], F32)
    Rf = P.sbuf("Rf", [128, 4, 256], F32)
    Rb = P.sbuf("Rb", [128, 4, 256], BF16)
    Sm = [P.sbuf("Sm%d" % i, [128, 4, 128], BF16) for i in range(2)]
    yrtok = [P.sbuf("yrtok%d" % i, [128, D], BF16) for i in range(2)]
    pinb = [P.sbuf("pinb%d" % i, [128, PLE], F32) for i in range(1)] * 2
    pb16 = P.sbuf("pb16", [128, PLE], BF16)
    ppT = P.sbuf("ppT", [128, 2, QB], BF16)

    banks = []
    for i in range(8):
        bf = P.psum("pb%d" % i, [128, 512], F32)
        bh = P.alias("pbh%d" % i, bf, bf.handle.bitcast(BF16).reshape([128, 8, 128]), [128, 8, 128], whole=True)
        banks.append((bf, bh))
    held = set()
    bank_ptr = [0]

    def next_bank():
        while True:
            i = bank_ptr[0] % 8
            bank_ptr[0] += 1
            if i not in held:
                return i

    def ap_of(v):
        return v.ap if isinstance(v, View) else v

    def vlist(*vs):
        return [v for v in vs if isinstance(v, View)]

    def mm(o, lhsT, rhs, start, stop):
        P.op("pe", lambda e: e.matmul(o.ap, lhsT=lhsT.ap, rhs=rhs.ap, start=start, stop=stop),
             reads=[lhsT, rhs], writes=[o])

    idv = ident.full()

    def tr(o, i_):
        P.op("pe", lambda e: e.transpose(out=o.ap, in_=i_.ap, identity=idv.ap), reads=[i_, idv], writes=[o])

    def act(o, i_, func, scale=1.0, bias=None, accum=None):
        kw = {}
        if bias is not None:
            kw["bias"] = ap_of(bias)
        if accum is not None:
            kw["accum_out"] = accum.ap
        P.op("act", lambda e: e.activation(out=o.ap, in_=i_.ap, func=func, scale=ap_of(scale), **kw),
             reads=vlist(i_, scale, bias), writes=vlist(o, accum))

    def tt(eng, o, a, b, op):
        P.op(eng, lambda e: e.tensor_tensor(out=o.ap, in0=a.ap, in1=b.ap, op=op), reads=[a, b], writes=[o])

    def ts(eng, o, a, s1, s2, op0, op1=None):
        if op1 is None:
            P.op(eng, lambda e: e.tensor_scalar(out=o.ap, in0=a.ap, scalar1=ap_of(s1), scalar2=None, op0=op0),
                 reads=vlist(a, s1), writes=[o])
        else:
            P.op(eng, lambda e: e.tensor_scalar(out=o.ap, in0=a.ap, scalar1=ap_of(s1), scalar2=ap_of(s2), op0=op0, op1=op1),
                 reads=vlist(a, s1, s2), writes=[o])

    def stt(eng, o, a, s, b, op0, op1):
        P.op(eng, lambda e: e.scalar_tensor_tensor(out=o.ap, in0=a.ap, scalar=ap_of(s), in1=b.ap, op0=op0, op1=op1),
             reads=vlist(a, s, b), writes=[o])

    def cp(eng, o, a):
        P.op(eng, lambda e: e.tensor_copy(out=o.ap, in_=a.ap), reads=[a], writes=[o])

    def memset(eng, o, val):
        P.op(eng, lambda e: e.memset(o.ap, val), writes=[o])

    def bcast(v, shape):
        return v.with_ap(lambda ap: ap.broadcast_to(shape))

    epsc = P.sbuf("epsc", [128, 1], F32)
    memset("pool", epsc.full(), EPS)
    memset("pool", tmpf[0][:, 0:128], 1.0)
    P.op("pool", lambda e: e.affine_select(out=tmpf[0][:, 0:128].ap, in_=tmpf[0][:, 0:128].ap, pattern=[[-1, 128]],
                                           compare_op=ALU.is_equal, fill=0.0, base=0, channel_multiplier=1),
         reads=[tmpf[0][:, 0:128]], writes=[tmpf[0][:, 0:128]])
    cp("dve", ident.full(), tmpf[0][:, 0:128])
    memset("pool", tmpf[0][:, 0:128], 1.0)
    cp("dve", ones.full(), tmpf[0][:, 0:128])
    P.op("pool", lambda e: e.affine_select(out=tmpf[0][:, 0:128].ap, in_=tmpf[0][:, 0:128].ap, pattern=[[1, 128]],
                                           compare_op=ALU.is_ge, fill=0.0, base=0, channel_multiplier=-1),
         reads=[tmpf[0][:, 0:128]], writes=[tmpf[0][:, 0:128]])
    cp("dve", trimask.full(), tmpf[0][:, 0:128])
    P.op("pool", lambda e: e.iota(iota_p.full().ap, pattern=[[0, 1]], base=0, channel_multiplier=1,
                                  allow_small_or_imprecise_dtypes=True), writes=[iota_p.full()])
    P.op("pool", lambda e: e.iota(tmpf[1][:, 0:128].ap, pattern=[[1, 128]], base=1, channel_multiplier=0,
                                  allow_small_or_imprecise_dtypes=True), writes=[tmpf[1][:, 0:128]])
    def bias_col(h, delta):
        di = delta // 128 + 3
        assert 0 <= di < 24
        return bias_tab[:, h * 24 + di: h * 24 + di + 1]
    for h in range(NH):
        for di in range(24):
            delta = 128 * (di - 3)
            ts("dve", bias_tab[:, h * 24 + di: h * 24 + di + 1], iota_p.full(), SLOPES[h],
               -SLOPES[h] * (delta + SUBW[h] // 2), ALU.mult, ALU.add)
    for h in range(4):
        act(qdec[:, h, :], tmpf[1][:, 0:128], AF.Exp, scale=LNG[h])
        act(kdec[:, h, :], tmpf[1][:, 0:128], AF.Exp, scale=-LNG[h], bias=None)
        ts("dve", kdec[:, h, :], kdec[:, h, :], 128.0 ** -0.5, None, ALU.mult)
        memset("pool", cdec[:, h:h + 1], GAM[h] ** 128)
    for i, gsrc in enumerate((g_mix, g_mlp, g_ple)):
        src = View(gsrc, gsrc.handle.ap().rearrange("(c p) -> p c", p=128), ((0, D),))
        P.dma("sp", gcol[:, i, :], src, allow_slow_non_contiguous=True)
    P.dma("sp", gdcol.full(), View(g_diff_sub, g_diff_sub.handle.ap().rearrange("(p o) -> p o", o=1), ((0, 128),)),
          allow_slow_non_contiguous=True)
    ts("dve", gdcol.full(), gdcol.full(), 1.0 - LAM_INIT, None, ALU.mult)
    P.dma("sp", gret_b.full(), View(g_ret_sub, g_ret_sub.handle.ap().partition_broadcast(128), ((0, D),)))
    ts("dve", gret_b.full(), gret_b.full(), 0.5, None, ALU.mult)
    P.dma("sp", gfin_b.full(), View(g_final, g_final.handle.ap().partition_broadcast(128), ((0, D),)))
    for i, lsrc in enumerate(lam_in):
        P.dma("sp", lamt[:, i, :], View(lsrc, lsrc.handle.ap().partition_broadcast(128), ((0, 64),)))
    tt("dve", lamt[:, 0, :], lamt[:, 0, :], lamt[:, 1, :], ALU.mult)
    tt("dve", lamt[:, 2, :], lamt[:, 2, :], lamt[:, 3, :], ALU.mult)
    P.op("dve", lambda e: e.reduce_sum(out=lams[:, 0:1].ap, in_=lamt[:, 0, :].ap, axis=AX.X), reads=[lamt[:, 0, :]], writes=[lams[:, 0:1]])
    P.op("dve", lambda e: e.reduce_sum(out=lams[:, 1:2].ap, in_=lamt[:, 2, :].ap, axis=AX.X), reads=[lamt[:, 2, :]], writes=[lams[:, 1:2]])
    act(lams[:, 2:4], lams[:, 0:2], AF.Exp)
    tt("dve", lams[:, 4:5], lams[:, 3:4], lams[:, 2:3], ALU.subtract)
    ts("dve", lams[:, 4:5], lams[:, 4:5], -LAM_INIT, None, ALU.add)
    neg_lam = lams[:, 4:5]

    conv_order = []
    for key in QB_SCHED:
        if key not in conv_order:
            conv_order.append(key)
    for key in conv_order:
        t, src, r0, c0 = WT[key]
        w = wsrc[src]
        if key[0] == "ple":
            for hf in range(2):
                sv = View(w, w.handle.ap()[:, hf * 512:(hf + 1) * 512].rearrange("(c p) n -> p c n", p=128), ((0, 1), (0, 1)))
                P.dma("pool", wb[t, :, 2 * hf:2 * hf + 2, :], sv)
        else:
            sv = View(w, w.handle.ap()[r0:r0 + 1024, c0:c0 + 512].rearrange("(c p) n -> p c n", p=128), ((0, 1), (0, 1)))
            P.dma("pool", wb[t], sv)

    sched = [WT[k][0] for _ in range(n_qb) for k in QB_SCHED]
    ws = {"issued": 0, "next": 0}

    def wget(expect_key, ahead=None):
        i = ws["next"]
        assert sched[i] == WT[expect_key][0], (expect_key, i)
        ahead = nslots - 1 if ahead is None else ahead
        while ws["issued"] < min(len(sched), i + 1 + ahead):
            j = ws["issued"]
            if sched[j] == WT[("ple", 0)][0]:
                P.dma("sp", wsl[j % nslots][:, 0:4, :], wb[sched[j], :, 0:4, :])
            else:
                P.dma("sp", wsl[j % nslots].full(), wb[sched[j]])
            ws["issued"] += 1
        ws["next"] += 1
        return wsl[i % nslots]

    cnt = {"evac": 0, "tmp": 0, "pt": 0}
    yraw_d = P.sbuf("yraw_d", [128, QB], F32)
    sqb_d = P.sbuf("sqb_d", [128, QB], BF16)
    ones_f = P.sbuf("ones_f", [128, 128], F32)
    memset("pool", ones_f.full(), 1.0)
    lnk = lams[:, 5:6]
    memset("pool", lnk, -8.0 * math.log(2.0))
    deferred = []

    def run_deferred(level):
        for d in list(deferred):
            if d[0] is not None:
                f = d[0]
                d[0] = None
                f()
            if level >= 2 and d[1] is not None:
                f = d[1]
                d[1] = None
                f()
            if d[0] is None and d[1] is None:
                deferred.remove(d)

    def tmp_half():
        i = cnt["tmp"] % 6
        cnt["tmp"] += 1
        return tmpf[i // 2][:, (i % 2) * 512:(i % 2 + 1) * 512]

    def tmp_full():
        i = (cnt["tmp"] + 1) // 2 % 3
        cnt["tmp"] = (i + 1) * 2
        return tmpf[i]

    def evac_copy(o, i_, scale=None):
        cnt["evac"] += 1
        if cnt["evac"] % 2:
            act(o, i_, AF.Copy, scale=1.0 if scale is None else scale)
        elif scale is None:
            cp("dve", o, i_)
        else:
            ts("dve", o, i_, scale, None, ALU.mult)

    def rsqrt_cols(o, ssq, n, inv_n):
        act(o, ssq, AF.Ln, scale=inv_n, bias=epsc[:, 0:1])
        act(o, o, AF.Exp, scale=-0.5)

    def rms_to_hT(src_tile, tcol, gi, scol):
        hbuf = hb[scol % 2]
        P.op("dve", lambda e: e.scalar_tensor_tensor(out=hbuf.full().ap, in0=src_tile.ap, scalar=1.0, in1=src_tile.ap,
                                                      op0=ALU.mult, op1=ALU.mult, accum_out=stat[:, scol:scol + 1].ap),
             reads=[src_tile], writes=[hbuf.full(), stat[:, scol:scol + 1]])
        rsqrt_cols(stat[:, scol + 8:scol + 9], stat[:, scol:scol + 1], 1, 1.0 / D)
        act(hbuf.full(), src_tile, AF.Copy, scale=stat[:, scol + 8:scol + 9])
        b = next_bank()
        for k in range(8):
            tr(banks[b][1][:, k, :], hbuf[:, k * 128:(k + 1) * 128])
        tt("dve", hT[:, :, tcol * 128:(tcol + 1) * 128], banks[b][1].full(),
           gcol[:, gi, :].with_ap(lambda ap: ap.unsqueeze(2).broadcast_to([128, 8, 128])), ALU.mult)

    U_QT, U_QR, U_KR, U_KRT, U_VR, U_YA, U_YR, U_GR = 0, 8, 12, 16, 20, 28, 36, 44
    U_MIX, U_TA = 0, 8

    for qb in range(n_qb):
        sq = qb // NQB_SEQ
        qi = qb % NQB_SEQ
        s0 = qi * QB
        t0 = sq * SEQ + s0

        for tti in range(4):
            xt = tmp_full()
            P.dma("sp", xt.full(), x[t0 + tti * 128: t0 + (tti + 1) * 128, :])
            rms_to_hT(xt.full(), tti, 0, tti % 2)

        if qi == 0:
            memset("pool", Rf.full(), 0.0)
            memset("pool", Rb.full(), 0.0)

        if stop <= 1:
            continue
        def proj_fm(key, dst_fn, post):
            wt = wget(key)
            for m in range(4):
                b = next_bank()
                for k in range(8):
                    mm(banks[b][0].full(), wt[:, k, m * 128:(m + 1) * 128], hT[:, k, :], k == 0, k == 7)
                post(m, banks[b][0].full())

        def proj_tm(key, post):
            wt = wget(key)
            for tti in range(4):
                b = next_bank()
                for k in range(8):
                    mm(banks[b][0].full(), hT[:, k, tti * 128:(tti + 1) * 128], wt[:, k, :], k == 0, k == 7)
                post(tti, banks[b][0].full())

        for j in range(2):
            proj_fm(("qa", j), None, lambda m, ps, j=j: evac_copy(U[:, U_QT + 4 * j + m, :], ps, 0.125))
        for j in range(2):
            proj_fm(("ka", j), None, lambda m, ps, j=j: evac_copy(KT[:, 4 * j + m, s0:s0 + QB], ps))
        for j in range(2):
            proj_tm(("va", j), lambda tti, ps, j=j: evac_copy(VC[:, qi * 4 + tti, j * 512:(j + 1) * 512], ps))
        qdec_b = qdec.full().with_ap(lambda ap: ap.unsqueeze(1).broadcast_to([128, 4, 4, 128]))
        def post_qr(m, ps):
            tt("dve", U[:, U_QR + m, :].with_ap(lambda ap: ap.rearrange("p (t n) -> p t n", n=128)),
               ps.with_ap(lambda ap: ap.rearrange("p (t n) -> p t n", n=128)),
               qdec[:, m, :].with_ap(lambda ap: ap.unsqueeze(1).broadcast_to([128, 4, 128])), ALU.mult)
        def post_kr(m, ps):
            tt("dve", U[:, U_KR + m, :].with_ap(lambda ap: ap.rearrange("p (t n) -> p t n", n=128)),
               ps.with_ap(lambda ap: ap.rearrange("p (t n) -> p t n", n=128)),
               kdec[:, m, :].with_ap(lambda ap: ap.unsqueeze(1).broadcast_to([128, 4, 128])), ALU.mult)
        proj_fm(("qr", 0), None, post_qr)
        proj_fm(("kr", 0), None, post_kr)
        for j in range(2):
            proj_tm(("vr", j), lambda tti, ps, j=j: evac_copy(U[:, U_VR + 2 * tti + j, :], ps))
        def post_gr(m, ps, j):
            th = tmp_half()
            act(th, ps, AF.Tanh, scale=0.5)
            stt("dve", U[:, U_GR + 4 * j + m, :], th, 1.0, ps, ALU.add, ALU.mult)
        for j in range(2):
            proj_fm(("gr", j), None, lambda m, ps, j=j: post_gr(m, ps, j))
        for tti in range(4):
            b = next_bank()
            for h in range(4):
                tr(banks[b][1][:, h, :], U[:, U_KR + h, tti * 128:(tti + 1) * 128])
            tt("dve", U[:, U_KRT + tti, :].with_ap(lambda ap: ap.rearrange("p (h n) -> p h n", n=128)),
               banks[b][1][:, 0:4, :], cdec.full().with_ap(lambda ap: ap.unsqueeze(2).broadcast_to([128, 4, 128])), ALU.mult)

        if stop <= 2:
            continue
        for h in range(NH):
            W = SUBW[h]
            slope = SLOPES[h]
            acc = []
            for _ in range(2):
                b = next_bank()
                held.add(b)
                acc.append(b)
            O = [banks[acc[0]][0], banks[acc[1]][0]]
            den = [tmp_half(), tmp_half()]
            nkt = qi * 4 + 4
            jlist = []
            for j in range(nkt):
                k0 = j * 128
                mind = max(0, s0 - (k0 + 127))
                if slope * mind > CUT:
                    continue
                jlist.append(j)
            staged = {}

            def stage(j):
                k0 = j * 128
                r = j - qi * 4
                a_lo = max(0, r) * 128
                Sb = []
                for c in range(2):
                    b = next_bank()
                    S = banks[b][0]
                    mm(S[:, a_lo:QB], KT[64 * c:64 * c + 64, h, k0:k0 + 128], U[64 * c:64 * c + 64, U_QT + h, a_lo:QB], True, True)
                    Sb.append(S)
                pts = []
                for c in range(2):
                    S = Sb[c]
                    pt = PT[cnt["pt"] % 4]
                    cnt["pt"] += 1
                    for sb0 in range(0, QB, W):
                        lo = max(sb0, a_lo)
                        hi = sb0 + W
                        if lo >= hi:
                            continue
                        delta = s0 + sb0 - k0
                        act(pt[:, lo:hi], S[:, lo:hi], AF.Exp, bias=bias_col(h, delta))
                    if r >= 0:
                        dsl = pt[:, a_lo:a_lo + 128]
                        P.op("pool", lambda e, dsl=dsl: e.affine_select(out=dsl.ap, in_=dsl.ap, pattern=[[1, 128]],
                                                                        compare_op=ALU.is_ge, fill=0.0, base=0, channel_multiplier=-1),
                             reads=[dsl], writes=[dsl])
                    pts.append(pt)
                staged[j] = (pts, a_lo)

            def consume(j):
                pts, a_lo = staged.pop(j)
                st = (j == jlist[0])
                last = (j == jlist[-1])
                assert a_lo == 0 or not st
                for c in range(2):
                    mm(O[c][:, a_lo:QB], VC[:, j, h * 128:(h + 1) * 128], pts[c][:, a_lo:QB], st, last)
                for c in range(2):
                    if st:
                        cp("dve", den[c], pts[c].full())
                    else:
                        tt("dve", den[c].cols(a_lo, QB), den[c].cols(a_lo, QB), pts[c][:, a_lo:QB], ALU.add)

            n_it = len(jlist)
            stage(jlist[0])
            run_deferred(1)
            for i in range(n_it):
                if i + 1 < n_it:
                    stage(jlist[i + 1])
                consume(jlist[i])
                if i == min(3, n_it - 1):
                    run_deferred(2)

            def part_a(O=O, den=den, acc=acc):
                for c in range(2):
                    b = next_bank()
                    mm(banks[b][0].full(), ones_f.full(), den[c], True, True)
                    act(den[c], banks[b][0].full(), AF.Ln, scale=2.0 ** -8)
                    act(den[c], den[c], AF.Exp, scale=-1.0, bias=lnk)
                tt("dve", den[0], O[0].full(), den[0], ALU.mult)
                tt("dve", den[1], O[1].full(), den[1], ALU.mult)
                for b in acc:
                    held.discard(b)
                stt("dve", yraw_d.full(), den[1], neg_lam, den[0], ALU.mult, ALU.add)
                act(sqb_d.full(), yraw_d.full(), AF.Square)

            def part_b(h=h):
                b = next_bank()
                mm(banks[b][0].full(), ones.full(), sqb_d.full(), True, True)
                rstd = tmp_half()
                act(rstd, banks[b][0].full(), AF.Ln, scale=1.0 / 128, bias=epsc[:, 0:1])
                act(rstd, rstd, AF.Exp, scale=-0.5)
                stt("dve", U[:, U_YA + h, :], yraw_d.full(), gdcol.full(), rstd, ALU.mult, ALU.mult)
            run_deferred(2)
            deferred.append([part_a, part_b])
            run_deferred(1)

        if stop <= 3:
            continue
        def ret_scores(tti):
            tsl = slice(tti * 128, (tti + 1) * 128)
            b = next_bank()
            for h in range(4):
                mm(banks[b][0][:, h * 128:(h + 1) * 128], U[:, U_KR + h, tsl], U[:, U_QR + h, tsl], True, True)
            sm = Sm[tti % 2]
            tt("dve", sm.full(), banks[b][0].full().with_ap(lambda ap: ap.rearrange("p (h n) -> p h n", n=128)),
               trimask.full().with_ap(lambda ap: ap.unsqueeze(1).broadcast_to([128, 4, 128])), ALU.mult)

        def ret_transposes(tti):
            tsl = slice(tti * 128, (tti + 1) * 128)
            yt = yrtok[tti % 2]
            b = next_bank()
            for k in range(8):
                tr(banks[b][1][:, k, :], yt[:, k * 128:(k + 1) * 128])
            tt("dve", U[:, U_YR:U_YR + 8, tsl], banks[b][1].full(), U[:, U_GR:U_GR + 8, tsl], ALU.mult)

        ret_scores(0)
        for tti in range(4):
            tsl = slice(tti * 128, (tti + 1) * 128)
            rb_ = [next_bank(), next_bank()]
            for h in range(4):
                o = banks[rb_[h // 2]][0][:, (h % 2) * 256:(h % 2 + 1) * 256]
                vr = U[:, U_VR + 2 * tti + h // 2, (h % 2) * 256:(h % 2 + 1) * 256]
                mm(o, U[:, U_KRT + tti, h * 128:(h + 1) * 128], vr, True, True)
            if tti < 3:
                ret_scores(tti + 1)
            if tti == 0:
                run_deferred(1)
            if tti == 1:
                run_deferred(2)
            sm = Sm[tti % 2]
            ob = [next_bank(), next_bank()]
            for h in range(4):
                o = banks[ob[h // 2]][0][:, (h % 2) * 256:(h % 2 + 1) * 256]
                vr = U[:, U_VR + 2 * tti + h // 2, (h % 2) * 256:(h % 2 + 1) * 256]
                mm(o, sm[:, h, :], vr, True, False)
                mm(o, U[:, U_QR + h, tsl], Rb[:, h, :], False, True)
            if tti > 0:
                ret_transposes(tti - 1)
            for h in range(4):
                o = banks[rb_[h // 2]][0][:, (h % 2) * 256:(h % 2 + 1) * 256]
                stt("dve", Rf[:, h, :], Rf[:, h, :], GAM[h] ** 128, o, ALU.mult, ALU.add)
            cp("dve", Rb.full(), Rf.full())
            yt = yrtok[tti % 2]
            sc = 4 * (tti % 2)
            junk = hb[tti % 2]
            for h in range(4):
                o = banks[ob[h // 2]][0][:, (h % 2) * 256:(h % 2 + 1) * 256]
                act(junk[:, h * 256:(h + 1) * 256], o, AF.Square, accum=stat[:, sc + h:sc + h + 1])
            rsqrt_cols(stat[:, sc + 8:sc + 12], stat[:, sc:sc + 4], 4, 1.0 / 256)
            for h in range(4):
                o = banks[ob[h // 2]][0][:, (h % 2) * 256:(h % 2 + 1) * 256]
                stt("dve", yt[:, h * 256:(h + 1) * 256], o, stat[:, sc + 8 + h:sc + 9 + h], gret_b[:, h * 256:(h + 1) * 256], ALU.mult, ALU.mult)
        ret_transposes(3)

        if stop <= 4:
            continue
        for tti in range(4):
            P.dma("sp", xres[:, tti, :], x[t0 + tti * 128: t0 + (tti + 1) * 128, :])

        for j in range(2):
            wt = wget(("ga", j))
            for m in range(4):
                b = next_bank()
                for k in range(8):
                    mm(banks[b][0].full(), wt[:, k, m * 128:(m + 1) * 128], hT[:, k, :], k == 0, k == 7)
                act(U[:, U_TA + m, :], banks[b][0].full(), AF.Tanh, scale=0.5)
            wt = wget(("wd", j))
            for m in range(4):
                b = next_bank()
                for k in range(8):
                    mm(banks[b][0].full(), wt[:, k, m * 128:(m + 1) * 128], U[:, U_YA + k, :], k == 0, k == 7)
                stt("dve", U[:, U_MIX + 4 * j + m, :], U[:, U_TA + m, :], 1.0, banks[b][0].full(), ALU.add, ALU.mult)
            wt = wget(("gb", j))
            for m in range(4):
                b = next_bank()
                for k in range(8):
                    mm(banks[b][0].full(), wt[:, k, m * 128:(m + 1) * 128], hT[:, k, :], k == 0, k == 7)
                act(U[:, U_TA + m, :], banks[b][0].full(), AF.Tanh, scale=0.5)
            wt = wget(("wr", j))
            for m in range(4):
                b = next_bank()
                for k in range(8):
                    mm(banks[b][0].full(), wt[:, k, m * 128:(m + 1) * 128], U[:, U_YR + k, :], k == 0, k == 7)
                th = tmp_half()
                stt("dve", th, U[:, U_TA + m, :], 1.0, banks[b][0].full(), ALU.add, ALU.mult)
                tt("dve", U[:, U_MIX + 4 * j + m, :], U[:, U_MIX + 4 * j + m, :], th, ALU.add)

        if stop <= 5:
            continue
        wts = [wget(("wo", 0)), wget(("wo", 1), ahead=0)]
        for tti in range(4):
            for n in range(2):
                wt = wts[n]
                b = next_bank()
                for k in range(8):
                    mm(banks[b][0].full(), U[:, U_MIX + k, tti * 128:(tti + 1) * 128], wt[:, k, :], k == 0, k == 7)
                xs = xres[:, tti, n * 512:(n + 1) * 512]
                stt("dve", xs, banks[b][0].full(), 0.5, xs, ALU.mult, ALU.add)
            if tti > 1:
                rms_to_hT(xres[:, tti - 2, :], tti - 2, 1, (tti - 2) % 2)
        rms_to_hT(xres[:, 2, :], 2, 1, 0)
        rms_to_hT(xres[:, 3, :], 3, 1, 1)

        if stop <= 7:
            continue

        if stop <= 7:
            continue
        for j in range(8):
            wt = wget(("f1", j))
            for m in range(4):
                b = next_bank()
                for k in range(8):
                    mm(banks[b][0].full(), wt[:, k, m * 128:(m + 1) * 128], hT[:, k, :], k == 0, k == 7)
                th = tmp_half()
                act(th, banks[b][0].full(), AF.Square)
                stt("dve", U[:, 4 * j + m, :], banks[b][0].full(), 0.0, th, ALU.is_gt, ALU.mult)

        if stop <= 8:
            continue
        for n in range(2):
            accb = []
            for _ in range(4):
                b = next_bank()
                held.add(b)
                accb.append(b)
            for g in range(4):
                wt = wget(("f2", n * 4 + g))
                for tti in range(4):
                    for k in range(8):
                        mm(banks[accb[tti]][0].full(), U[:, g * 8 + k, tti * 128:(tti + 1) * 128], wt[:, k, :],
                           g == 0 and k == 0, g == 3 and k == 7)
            for tti in range(4):
                xs = xres[:, tti, n * 512:(n + 1) * 512]
                tt("dve", xs, banks[accb[tti]][0].full(), xs, ALU.add)
                held.discard(accb[tti])

        if stop <= 9:
            continue
        for tti in range(4):
            rms_to_hT(xres[:, tti, :], tti, 2, tti % 2)
        wt = wget(("ple", 0))
        for tti in range(4):
            pi = pinb[tti % 2]
            P.dma("sp", pi.full(), pin[t0 + tti * 128: t0 + (tti + 1) * 128, :])
            cp("pool", pb16.full(), pi.full())
            b = next_bank()
            for kc in range(2):
                tr(banks[b][1][:, kc, :], pb16[:, kc * 128:(kc + 1) * 128])
            cp("dve", ppT[:, :, tti * 128:(tti + 1) * 128], banks[b][1][:, 0:2, :])
            for n in range(2):
                b = next_bank()
                for kc in range(2):
                    mm(banks[b][0].full(), ppT[:, kc, tti * 128:(tti + 1) * 128], wt[:, 2 * n + kc, :], kc == 0, kc == 1)
                evac_copy(U[:, U_YA + 2 * tti + n, :], banks[b][0].full())
        for n in range(2):
            wt = wget(("pg", n))
            for tti in range(4):
                b = next_bank()
                for k in range(8):
                    mm(banks[b][0].full(), hT[:, k, tti * 128:(tti + 1) * 128], wt[:, k, :], k == 0, k == 7)
                th = tmp_half()
                act(th, banks[b][0].full(), AF.Tanh, scale=0.5)
                stt("dve", th, th, 1.0, U[:, U_YA + 2 * tti + n, :], ALU.add, ALU.mult)
                xs = xres[:, tti, n * 512:(n + 1) * 512]
                stt("dve", xs, th, 0.5, xs, ALU.mult, ALU.add)
        for tti in range(4):
            sc = tti % 2
            junk = hb[sc]
            act(junk.full(), xres[:, tti, :], AF.Square, accum=stat[:, sc:sc + 1])
            rsqrt_cols(stat[:, sc + 8:sc + 9], stat[:, sc:sc + 1], 1, 1.0 / D)
            ot = tmp_full()
            stt("dve", ot.full(), xres[:, tti, :], stat[:, sc + 8:sc + 9], gfin_b.full(), ALU.mult, ALU.mult)
            P.dma("pool", out[t0 + tti * 128: t0 + (tti + 1) * 128, :], ot.full())

    P.barrier("pool", [out.full()])
    P.barrier("sp", [out.full()])
    P.emit()
    P.close()
    return nc, P


_CACHE = {}


def kernel(**inputs):
    n = 8
    xs = np.ascontiguousarray(np.asarray(inputs["x"], dtype=np.float32)).reshape(n, TOK, D)
    ps = np.ascontiguousarray(np.asarray(inputs["p"], dtype=np.float32)).reshape(n, TOK, PLE)
    shared = {}
    for k in ("g_mix", "g_mlp", "g_ple", "w_in", "w_branch_diff", "w_branch_ret", "w_out", "w_ff1", "w_ff2",
              "w_ple_gate", "w_ple", "lam_q1", "lam_k1", "lam_q2", "lam_k2", "g_diff_sub"):
        a = np.asarray(inputs[k], dtype=np.float32)
        shared[k] = np.ascontiguousarray(a.reshape(a.shape[1:]))
    shared["g_ret_sub"] = np.ascontiguousarray(np.asarray(inputs["g_ret_sub"], dtype=np.float32).reshape(1024))
    shared["g_final"] = np.ascontiguousarray(np.asarray(inputs["g_final"], dtype=np.float32))
    if "nc" not in _CACHE:
        _CACHE["nc"] = build_program()[0]
    nc = _CACHE["nc"]
    in_maps = [dict(shared, x=xs[i], p=ps[i]) for i in range(n)]
    res = run_bass_kernel_spmd(nc, in_maps, core_ids=list(range(n)))
    outs = [np.asarray(res.results[i]["out"], dtype=np.float32).reshape(NSEQ, SEQ, D) for i in range(n)]
    return np.concatenate(outs, axis=0)
```

```python
import contextlib
import math
import numpy as np
import concourse.bass as bass
import concourse.mybir as mybir
from concourse.bass_utils import run_bass_kernel_spmd

F32 = mybir.dt.float32
BF16 = mybir.dt.bfloat16
AF = mybir.ActivationFunctionType
ALU = mybir.AluOpType
AX = mybir.AxisListType


def _norm_idx(idx, shape):
    if not isinstance(idx, tuple):
        idx = (idx,)
    box = []
    for d, n in enumerate(shape):
        if d < len(idx):
            i = idx[d]
            if isinstance(i, slice):
                lo = 0 if i.start is None else i.start
                hi = n if i.stop is None else i.stop
            else:
                lo, hi = int(i), int(i) + 1
        else:
            lo, hi = 0, n
        assert 0 <= lo < hi <= n, (idx, shape)
        box.append((lo, hi))
    return tuple(box)


class View:
    __slots__ = ("buf", "ap", "box")

    def __init__(self, buf, ap, box):
        self.buf, self.ap, self.box = buf, ap, box

    def with_ap(self, fn):
        return View(self.buf, fn(self.ap), self.box)

    def cols(self, lo, hi):
        b0 = self.box[-1][0]
        return View(self.buf, self.ap[:, lo:hi], self.box[:-1] + ((b0 + lo, b0 + hi),))


class Buf:
    def __init__(self, prog, name, handle, shape, tracked=True, whole=False):
        self.prog, self.name, self.handle, self.shape = prog, name, handle, tuple(shape)
        self.tracked = tracked
        self.whole = whole
        self.group = None
        self.hist = []

    def __getitem__(self, idx):
        box = _norm_idx(idx, self.shape)
        if self.whole:
            box = tuple((0, n) for n in self.shape)
        return View(self, self.handle[idx], box)

    def full(self):
        return self[tuple(slice(None) for _ in self.shape)]


def _overlap(a, b):
    for (l0, h0), (l1, h1) in zip(a, b):
        if h0 <= l1 or h1 <= l0:
            return False
    return True


def _contains(outer, inner):
    for (l0, h0), (l1, h1) in zip(outer, inner):
        if l1 < l0 or h1 > h0:
            return False
    return True


class Instr:
    __slots__ = ("eng", "fn", "clock", "pos", "waits", "signal", "is_dma", "snap")


ENGS = ("pe", "act", "dve", "pool", "sp")
DMA_SLOTS = {"sp": 8, "act": 4, "pool": 8}


class Prog:
    def __init__(self, nc):
        self.nc = nc
        self.stack = contextlib.ExitStack()
        self.instrs = {e: [] for e in ENGS}
        self.known = {e: {} for e in ENGS}
        self.clock_pos = {}
        self.clock_instrs = {}
        self.dma_n = {e: 0 for e in DMA_SLOTS}
        self.n_total = 0

    def sbuf(self, name, shape, dtype):
        h = self.stack.enter_context(self.nc.sbuf_tensor(name, list(shape), dtype))
        return Buf(self, name, h, shape)

    def psum(self, name, shape, dtype=F32):
        h = self.stack.enter_context(self.nc.psum_tensor(name, list(shape), dtype))
        return Buf(self, name, h, shape, whole=True)

    def dram(self, name, shape, dtype, kind="Internal", tracked=True):
        h = self.nc.dram_tensor(name, list(shape), dtype, kind=kind)
        return Buf(self, name, h, shape, tracked=tracked)

    def alias(self, name, base, new_handle, shape, whole=False):
        b = Buf(self, name, new_handle, shape, whole=whole)
        if base.group is None:
            base.group = [base]
        base.group.append(b)
        b.group = base.group
        return b

    def _deps_for(self, view, is_write, deps, eng):
        buf = view.buf
        if not buf.tracked:
            return
        bufs = buf.group if buf.group is not None else (buf,)
        for b in bufs:
            same = b is buf
            for ebox, ewrite, eclock, epos in b.hist:
                if not (is_write or ewrite):
                    continue
                if same and not _overlap(ebox, view.box):
                    continue
                if eclock == "pe" and eng == "pe":
                    continue
                if deps.get(eclock, 0) < epos:
                    deps[eclock] = epos

    def _record(self, view, is_write, clock, pos, in_order):
        buf = view.buf
        if not buf.tracked:
            return
        box = view.box
        h = buf.hist
        if is_write:
            h[:] = [e for e in h if not _contains(box, e[0])]
            if buf.group is not None and buf.whole:
                for b in buf.group:
                    if b is not buf:
                        b.hist[:] = []
        elif in_order:
            h[:] = [e for e in h if not (e[2] == clock and not e[1] and _contains(box, e[0]))]
        h.append((box, is_write, clock, pos))

    def op(self, eng, fn, reads=(), writes=(), dma=False):
        ins = Instr()
        ins.eng, ins.fn, ins.is_dma, ins.signal = eng, fn, dma, dma
        if dma:
            n = self.dma_n[eng]
            self.dma_n[eng] = n + 1
            clock = ("dma", eng, n % DMA_SLOTS[eng])
        else:
            clock = eng
        pos = self.clock_pos.get(clock, 0) + 1
        self.clock_pos[clock] = pos
        self.clock_instrs.setdefault(clock, []).append(ins)
        ins.clock, ins.pos = clock, pos
        deps = {}
        if dma and pos > 1:
            deps[clock] = pos - 1
        for v in reads:
            self._deps_for(v, False, deps, eng)
        for v in writes:
            self._deps_for(v, True, deps, eng)
        known = self.known[eng]
        waits = []
        for ck, p in deps.items():
            if known.get(ck, 0) >= p:
                continue
            waits.append((ck, p))
            j = self.clock_instrs[ck][p - 1]
            j.signal = True
            for k2, p2 in j.snap.items():
                if known.get(k2, 0) < p2:
                    known[k2] = p2
            known[ck] = p
        ins.waits = waits
        ins.snap = dict(known)
        if fn is not None:
            for v in reads:
                self._record(v, False, clock, pos, not dma)
            for v in writes:
                self._record(v, True, clock, pos, not dma)
        self.instrs[eng].append(ins)
        self.n_total += 1
        return ins

    def dma(self, queue, out, in_, **kw):
        def fn(e):
            return e.dma_start(out=out.ap, in_=in_.ap, **kw)
        return self.op(queue, fn, reads=[in_], writes=[out], dma=True)

    def barrier(self, eng, views):
        return self.op(eng, None, reads=(), writes=views)

    def emit(self):
        nc = self.nc
        SEM_EPOCH = 1024
        sems = {}
        val = {}
        semof = {}
        for ck, lst in self.clock_instrs.items():
            nm = ck if isinstance(ck, str) else "d_%s_%d" % (ck[1], ck[2])
            step = 1 if isinstance(ck, str) else 16
            c = 0
            for ins in lst:
                if ins.signal:
                    ep, r = divmod(c, SEM_EPOCH // step)
                    c += 1
                    if (ck, ep) not in sems:
                        sems[(ck, ep)] = self.stack.enter_context(nc.semaphore("s_%s_%d" % (nm, ep)))
                    val[(ck, ins.pos)] = (r + 1) * step
                    semof[(ck, ins.pos)] = sems[(ck, ep)]
        engmap = {"pe": "tensor", "act": "scalar", "dve": "vector", "pool": "gpsimd", "sp": "sync"}
        with nc.Block() as block:
            for ename in ENGS:
                lst = self.instrs[ename]

                def body(e, lst=lst):
                    for ins in lst:
                        for ck, p in ins.waits:
                            e.wait_ge(semof[(ck, p)], val[(ck, p)])
                        if ins.fn is None:
                            continue
                        r = ins.fn(e)
                        if ins.signal:
                            r.then_inc(semof[(ins.clock, ins.pos)], 16 if ins.is_dma else 1)

                getattr(block, engmap[ename])(body)

    def close(self):
        self.stack.close()


D = 1024
SEQ = 2048
NSEQ = 4
TOK = NSEQ * SEQ
QB = 512
NQB_SEQ = SEQ // QB
PLE = 256
DFF = 4096
EPS = 1e-6
LAM_INIT = 0.8 - 0.6 * math.exp(-0.3 * 0)
NH = 8
SLOPES = [2.0 ** (-(h + 1)) for h in range(NH)]
SUBW = [128, 256, 512, 512, 512, 512, 512, 512]
GAM = [1.0 - 2.0 ** (-5.0 - h) for h in range(4)]
LNG = [math.log(g) for g in GAM]
CUT = 60.0

WT = {}
_t = 0
for nm, c0 in (("qa", 0), ("ka", 1024), ("va", 2048)):
    for j in range(2):
        WT[(nm, j)] = (_t, "w_in", 0, c0 + 512 * j); _t += 1
WT[("qr", 0)] = (_t, "w_in", 0, 3072); _t += 1
WT[("kr", 0)] = (_t, "w_in", 0, 3584); _t += 1
for nm, c0 in (("vr", 4096), ("gr", 5120), ("ga", 6144), ("gb", 7168)):
    for j in range(2):
        WT[(nm, j)] = (_t, "w_in", 0, c0 + 512 * j); _t += 1
for nm, src in (("wd", "w_branch_diff"), ("wr", "w_branch_ret"), ("wo", "w_out")):
    for j in range(2):
        WT[(nm, j)] = (_t, src, 0, 512 * j); _t += 1
for j in range(8):
    WT[("f1", j)] = (_t, "w_ff1", 0, 512 * j); _t += 1
for n in range(2):
    for g in range(4):
        WT[("f2", n * 4 + g)] = (_t, "w_ff2", 1024 * g, 512 * n); _t += 1
for j in range(2):
    WT[("pg", j)] = (_t, "w_ple_gate", 0, 512 * j); _t += 1
WT[("ple", 0)] = (_t, "w_ple", 0, 0); _t += 1
NWT = _t

QB_SCHED = ([("qa", 0), ("qa", 1), ("ka", 0), ("ka", 1), ("va", 0), ("va", 1), ("qr", 0), ("kr", 0),
             ("vr", 0), ("vr", 1), ("gr", 0), ("gr", 1)]
            + [("ga", 0), ("wd", 0), ("gb", 0), ("wr", 0), ("ga", 1), ("wd", 1), ("gb", 1), ("wr", 1)]
            + [("wo", 0), ("wo", 1)]
            + [("f1", j) for j in range(8)]
            + [("f2", j) for j in range(8)]
            + [("ple", 0), ("pg", 0), ("pg", 1)])


def build_program(n_qb=16, nslots=2, stop=99):
    nc = bass.Bass("TRN2", target_bir_lowering=False)
    P = Prog(nc)
    ext = lambda n, s: P.dram(n, s, F32, kind="ExternalInput", tracked=False)
    x = ext("x", [TOK, D])
    pin = ext("p", [TOK, PLE])
    g_mix = ext("g_mix", [D]); g_mlp = ext("g_mlp", [D]); g_ple = ext("g_ple", [D]); g_final = ext("g_final", [D])
    wsrc = {"w_in": ext("w_in", [D, 8192]), "w_branch_diff": ext("w_branch_diff", [D, D]),
            "w_branch_ret": ext("w_branch_ret", [D, D]), "w_out": ext("w_out", [D, D]),
            "w_ff1": ext("w_ff1", [D, DFF]), "w_ff2": ext("w_ff2", [DFF, D]),
            "w_ple_gate": ext("w_ple_gate", [D, D]), "w_ple": ext("w_ple", [PLE, D])}
    lam_in = [ext(n, [64]) for n in ("lam_q1", "lam_k1", "lam_q2", "lam_k2")]
    g_diff_sub = ext("g_diff_sub", [128])
    g_ret_sub = ext("g_ret_sub", [1024])
    out = P.dram("out", [TOK, D], F32, kind="ExternalOutput")
    wb = P.dram("wb", [NWT, 128, 8, 512], BF16)

    KT = P.sbuf("KT", [128, NH, SEQ], BF16)
    VC = P.sbuf("VC", [128, SEQ // 128, D], BF16)
    wsl = [P.sbuf("wsl%d" % i, [128, 8, 512], BF16) for i in range(nslots)]
    xres = P.sbuf("xres", [128, 4, D], F32)
    hT = P.sbuf("hT", [128, 8, QB], BF16)
    U = P.sbuf("U", [128, 52, QB], BF16)
    QT = lambda h: U[:, h, :]
    tmpf = [P.sbuf("tmpf%d" % i, [128, D], F32) for i in range(3)]
    PT = [P.sbuf("PT%d" % i, [128, QB], BF16) for i in range(4)]
    hb = [P.sbuf("hb%d" % i, [128, D], BF16) for i in range(1)] * 2
    ident = P.sbuf("ident", [128, 128], BF16)
    ones = P.sbuf("ones", [128, 128], BF16)
    iota_p = P.sbuf("iota_p", [128, 1], F32)
    mhalf = P.sbuf("mhalf", [128, 4], F32)
    bias_tab = P.sbuf("bias_tab", [128, NH * 24], F32)
    trimask = P.sbuf("trimask", [128, 128], BF16)
    qdec = P.sbuf("qdec", [128, 4, 128], F32)
    kdec = P.sbuf("kdec", [128, 4, 128], F32)
    cdec = P.sbuf("cdec", [128, 4], F32)
    gcol = P.sbuf("gcol", [128, 3, 8], F32)
    gdcol = P.sbuf("gdcol", [128, 1], F32)
    gret_b = P.sbuf("gret_b", [128, D], F32)
    gfin_b = P.sbuf("gfin_b", [128, D], F32)
    lamt = P.sbuf("lamt", [128, 4, 64], F32)
    lams = P.sbuf("lams", [128, 8], F32)
    stat = P.sbuf("stat", [128, 16], F32)
    Rf = P.sbuf("Rf", [128, 4, 256], F32)
    Rb = P.sbuf("Rb", [128, 4, 256], BF16)
    Sm = [P.sbuf("Sm%d" % i, [128, 4, 128], BF16) for i in range(2)]
    yrtok = [P.sbuf("yrtok%d" % i, [128, D], BF16) for i in range(2)]
    pinb = [P.sbuf("pinb%d" % i, [128, PLE], F32) for i in range(1)] * 2
    pb16 = P.sbuf("pb16", [128, PLE], BF16)
    ppT = P.sbuf("ppT", [128, 2, QB], BF16)

    banks = []
    for i in range(8):
        bf = P.psum("pb%d" % i, [128, 512], F32)
        bh = P.alias("pbh%d" % i, bf, bf.handle.bitcast(BF16).reshape([128, 8, 128]), [128, 8, 128], whole=True)
        banks.append((bf, bh))
    held = set()
    bank_ptr = [0]

    def next_bank():
        while True:
            i = bank_ptr[0] % 8
            bank_ptr[0] += 1
            if i not in held:
                return i

    def ap_of(v):
        return v.ap if isinstance(v, View) else v

    def vlist(*vs):
        return [v for v in vs if isinstance(v, View)]

    def mm(o, lhsT, rhs, start, stop):
        P.op("pe", lambda e: e.matmul(o.ap, lhsT=lhsT.ap, rhs=rhs.ap, start=start, stop=stop),
             reads=[lhsT, rhs], writes=[o])

    idv = ident.full()

    def tr(o, i_):
        P.op("pe", lambda e: e.transpose(out=o.ap, in_=i_.ap, identity=idv.ap), reads=[i_, idv], writes=[o])

    def act(o, i_, func, scale=1.0, bias=None, accum=None):
        kw = {}
        if bias is not None:
            kw["bias"] = ap_of(bias)
        if accum is not None:
            kw["accum_out"] = accum.ap
        P.op("act", lambda e: e.activation(out=o.ap, in_=i_.ap, func=func, scale=ap_of(scale), **kw),
             reads=vlist(i_, scale, bias), writes=vlist(o, accum))

    def tt(eng, o, a, b, op):
        P.op(eng, lambda e: e.tensor_tensor(out=o.ap, in0=a.ap, in1=b.ap, op=op), reads=[a, b], writes=[o])

    def ts(eng, o, a, s1, s2, op0, op1=None):
        if op1 is None:
            P.op(eng, lambda e: e.tensor_scalar(out=o.ap, in0=a.ap, scalar1=ap_of(s1), scalar2=None, op0=op0),
                 reads=vlist(a, s1), writes=[o])
        else:
            P.op(eng, lambda e: e.tensor_scalar(out=o.ap, in0=a.ap, scalar1=ap_of(s1), scalar2=ap_of(s2), op0=op0, op1=op1),
                 reads=vlist(a, s1, s2), writes=[o])

    def stt(eng, o, a, s, b, op0, op1):
        P.op(eng, lambda e: e.scalar_tensor_tensor(out=o.ap, in0=a.ap, scalar=ap_of(s), in1=b.ap, op0=op0, op1=op1),
             reads=vlist(a, s, b), writes=[o])

    def cp(eng, o, a):
        P.op(eng, lambda e: e.tensor_copy(out=o.ap, in_=a.ap), reads=[a], writes=[o])

    def memset(eng, o, val):
        P.op(eng, lambda e: e.memset(o.ap, val), writes=[o])

    def bcast(v, shape):
        return v.with_ap(lambda ap: ap.broadcast_to(shape))

    epsc = P.sbuf("epsc", [128, 1], F32)
    memset("pool", epsc.full(), EPS)
    memset("pool", tmpf[0][:, 0:128], 1.0)
    P.op("pool", lambda e: e.affine_select(out=tmpf[0][:, 0:128].ap, in_=tmpf[0][:, 0:128].ap, pattern=[[-1, 128]],
                                           compare_op=ALU.is_equal, fill=0.0, base=0, channel_multiplier=1),
         reads=[tmpf[0][:, 0:128]], writes=[tmpf[0][:, 0:128]])
    cp("dve", ident.full(), tmpf[0][:, 0:128])
    memset("pool", tmpf[0][:, 0:128], 1.0)
    cp("dve", ones.full(), tmpf[0][:, 0:128])
    P.op("pool", lambda e: e.affine_select(out=tmpf[0][:, 0:128].ap, in_=tmpf[0][:, 0:128].ap, pattern=[[1, 128]],
                                           compare_op=ALU.is_ge, fill=0.0, base=0, channel_multiplier=-1),
         reads=[tmpf[0][:, 0:128]], writes=[tmpf[0][:, 0:128]])
    cp("dve", trimask.full(), tmpf[0][:, 0:128])
    P.op("pool", lambda e: e.iota(iota_p.full().ap, pattern=[[0, 1]], base=0, channel_multiplier=1,
                                  allow_small_or_imprecise_dtypes=True), writes=[iota_p.full()])
    P.op("pool", lambda e: e.iota(tmpf[1][:, 0:128].ap, pattern=[[1, 128]], base=1, channel_multiplier=0,
                                  allow_small_or_imprecise_dtypes=True), writes=[tmpf[1][:, 0:128]])
    def bias_col(h, delta):
        di = delta // 128 + 3
        assert 0 <= di < 24
        return bias_tab[:, h * 24 + di: h * 24 + di + 1]
    for h in range(NH):
        for di in range(24):
            delta = 128 * (di - 3)
            ts("dve", bias_tab[:, h * 24 + di: h * 24 + di + 1], iota_p.full(), SLOPES[h],
               -SLOPES[h] * (delta + SUBW[h] // 2), ALU.mult, ALU.add)
    for h in range(4):
        act(qdec[:, h, :], tmpf[1][:, 0:128], AF.Exp, scale=LNG[h])
        act(kdec[:, h, :], tmpf[1][:, 0:128], AF.Exp, scale=-LNG[h], bias=None)
        ts("dve", kdec[:, h, :], kdec[:, h, :], 128.0 ** -0.5, None, ALU.mult)
        memset("pool", cdec[:, h:h + 1], GAM[h] ** 128)
    for i, gsrc in enumerate((g_mix, g_mlp, g_ple)):
        src = View(gsrc, gsrc.handle.ap().rearrange("(c p) -> p c", p=128), ((0, D),))
        P.dma("sp", gcol[:, i, :], src, allow_slow_non_contiguous=True)
    P.dma("sp", gdcol.full(), View(g_diff_sub, g_diff_sub.handle.ap().rearrange("(p o) -> p o", o=1), ((0, 128),)),
          allow_slow_non_contiguous=True)
    ts("dve", gdcol.full(), gdcol.full(), 1.0 - LAM_INIT, None, ALU.mult)
    P.dma("sp", gret_b.full(), View(g_ret_sub, g_ret_sub.handle.ap().partition_broadcast(128), ((0, D),)))
    ts("dve", gret_b.full(), gret_b.full(), 0.5, None, ALU.mult)
    P.dma("sp", gfin_b.full(), View(g_final, g_final.handle.ap().partition_broadcast(128), ((0, D),)))
    for i, lsrc in enumerate(lam_in):
        P.dma("sp", lamt[:, i, :], View(lsrc, lsrc.handle.ap().partition_broadcast(128), ((0, 64),)))
    tt("dve", lamt[:, 0, :], lamt[:, 0, :], lamt[:, 1, :], ALU.mult)
    tt("dve", lamt[:, 2, :], lamt[:, 2, :], lamt[:, 3, :], ALU.mult)
    P.op("dve", lambda e: e.reduce_sum(out=lams[:, 0:1].ap, in_=lamt[:, 0, :].ap, axis=AX.X), reads=[lamt[:, 0, :]], writes=[lams[:, 0:1]])
    P.op("dve", lambda e: e.reduce_sum(out=lams[:, 1:2].ap, in_=lamt[:, 2, :].ap, axis=AX.X), reads=[lamt[:, 2, :]], writes=[lams[:, 1:2]])
    act(lams[:, 2:4], lams[:, 0:2], AF.Exp)
    tt("dve", lams[:, 4:5], lams[:, 3:4], lams[:, 2:3], ALU.subtract)
    ts("dve", lams[:, 4:5], lams[:, 4:5], -LAM_INIT, None, ALU.add)
    neg_lam = lams[:, 4:5]

    conv_order = []
    for key in QB_SCHED:
        if key not in conv_order:
            conv_order.append(key)
    for key in conv_order:
        t, src, r0, c0 = WT[key]
        w = wsrc[src]
        if key[0] == "ple":
            for hf in range(2):
                sv = View(w, w.handle.ap()[:, hf * 512:(hf + 1) * 512].rearrange("(c p) n -> p c n", p=128), ((0, 1), (0, 1)))
                P.dma("pool", wb[t, :, 2 * hf:2 * hf + 2, :], sv)
        else:
            sv = View(w, w.handle.ap()[r0:r0 + 1024, c0:c0 + 512].rearrange("(c p) n -> p c n", p=128), ((0, 1), (0, 1)))
            P.dma("pool", wb[t], sv)

    sched = [WT[k][0] for _ in range(n_qb) for k in QB_SCHED]
    ws = {"issued": 0, "next": 0}

    def wget(expect_key, ahead=None):
        i = ws["next"]
        assert sched[i] == WT[expect_key][0], (expect_key, i)
        ahead = nslots - 1 if ahead is None else ahead
        while ws["issued"] < min(len(sched), i + 1 + ahead):
            j = ws["issued"]
            if sched[j] == WT[("ple", 0)][0]:
                P.dma("sp", wsl[j % nslots][:, 0:4, :], wb[sched[j], :, 0:4, :])
            else:
                P.dma("sp", wsl[j % nslots].full(), wb[sched[j]])
            ws["issued"] += 1
        ws["next"] += 1
        return wsl[i % nslots]

    cnt = {"evac": 0, "tmp": 0, "pt": 0}
    yraw_d = P.sbuf("yraw_d", [128, QB], F32)
    sqb_d = P.sbuf("sqb_d", [128, QB], BF16)
    ones_f = P.sbuf("ones_f", [128, 128], F32)
    memset("pool", ones_f.full(), 1.0)
    lnk = lams[:, 5:6]
    memset("pool", lnk, -8.0 * math.log(2.0))
    deferred = []

    def run_deferred(level):
        for d in list(deferred):
            if d[0] is not None:
                f = d[0]
                d[0] = None
                f()
            if level >= 2 and d[1] is not None:
                f = d[1]
                d[1] = None
                f()
            if d[0] is None and d[1] is None:
                deferred.remove(d)

    def tmp_half():
        i = cnt["tmp"] % 6
        cnt["tmp"] += 1
        return tmpf[i // 2][:, (i % 2) * 512:(i % 2 + 1) * 512]

    def tmp_full():
        i = (cnt["tmp"] + 1) // 2 % 3
        cnt["tmp"] = (i + 1) * 2
        return tmpf[i]

    def evac_copy(o, i_, scale=None):
        cnt["evac"] += 1
        if cnt["evac"] % 2:
            act(o, i_, AF.Copy, scale=1.0 if scale is None else scale)
        elif scale is None:
            cp("dve", o, i_)
        else:
            ts("dve", o, i_, scale, None, ALU.mult)

    def rsqrt_cols(o, ssq, n, inv_n):
        act(o, ssq, AF.Ln, scale=inv_n, bias=epsc[:, 0:1])
        act(o, o, AF.Exp, scale=-0.5)

    def rms_to_hT(src_tile, tcol, gi, scol):
        hbuf = hb[scol % 2]
        act(hbuf.full(), src_tile, AF.Square, accum=stat[:, scol:scol + 1])
        rsqrt_cols(stat[:, scol + 8:scol + 9], stat[:, scol:scol + 1], 1, 1.0 / D)
        act(hbuf.full(), src_tile, AF.Copy, scale=stat[:, scol + 8:scol + 9])
        b = next_bank()
        for k in range(8):
            tr(banks[b][1][:, k, :], hbuf[:, k * 128:(k + 1) * 128])
        tt("dve", hT[:, :, tcol * 128:(tcol + 1) * 128], banks[b][1].full(),
           gcol[:, gi, :].with_ap(lambda ap: ap.unsqueeze(2).broadcast_to([128, 8, 128])), ALU.mult)

    U_QT, U_QR, U_KR, U_KRT, U_VR, U_YA, U_YR, U_GR = 0, 8, 12, 16, 20, 28, 36, 44
    U_MIX, U_TA = 0, 8

    for qb in range(n_qb):
        sq = qb // NQB_SEQ
        qi = qb % NQB_SEQ
        s0 = qi * QB
        t0 = sq * SEQ + s0

        for tti in range(4):
            xt = tmp_full()
            P.dma("sp", xt.full(), x[t0 + tti * 128: t0 + (tti + 1) * 128, :])
            rms_to_hT(xt.full(), tti, 0, tti % 2)

        if qi == 0:
            memset("pool", Rf.full(), 0.0)
            memset("pool", Rb.full(), 0.0)

        if stop <= 1:
            continue
        def proj_fm(key, dst_fn, post):
            wt = wget(key)
            for m in range(4):
                b = next_bank()
                for k in range(8):
                    mm(banks[b][0].full(), wt[:, k, m * 128:(m + 1) * 128], hT[:, k, :], k == 0, k == 7)
                post(m, banks[b][0].full())

        def proj_tm(key, post):
            wt = wget(key)
            for tti in range(4):
                b = next_bank()
                for k in range(8):
                    mm(banks[b][0].full(), hT[:, k, tti * 128:(tti + 1) * 128], wt[:, k, :], k == 0, k == 7)
                post(tti, banks[b][0].full())

        for j in range(2):
            proj_fm(("qa", j), None, lambda m, ps, j=j: evac_copy(U[:, U_QT + 4 * j + m, :], ps, 0.125))
        for j in range(2):
            proj_fm(("ka", j), None, lambda m, ps, j=j: evac_copy(KT[:, 4 * j + m, s0:s0 + QB], ps))
        for j in range(2):
            proj_tm(("va", j), lambda tti, ps, j=j: evac_copy(VC[:, qi * 4 + tti, j * 512:(j + 1) * 512], ps))
        qdec_b = qdec.full().with_ap(lambda ap: ap.unsqueeze(1).broadcast_to([128, 4, 4, 128]))
        def post_qr(m, ps):
            tt("dve", U[:, U_QR + m, :].with_ap(lambda ap: ap.rearrange("p (t n) -> p t n", n=128)),
               ps.with_ap(lambda ap: ap.rearrange("p (t n) -> p t n", n=128)),
               qdec[:, m, :].with_ap(lambda ap: ap.unsqueeze(1).broadcast_to([128, 4, 128])), ALU.mult)
        def post_kr(m, ps):
            tt("dve", U[:, U_KR + m, :].with_ap(lambda ap: ap.rearrange("p (t n) -> p t n", n=128)),
               ps.with_ap(lambda ap: ap.rearrange("p (t n) -> p t n", n=128)),
               kdec[:, m, :].with_ap(lambda ap: ap.unsqueeze(1).broadcast_to([128, 4, 128])), ALU.mult)
        proj_fm(("qr", 0), None, post_qr)
        proj_fm(("kr", 0), None, post_kr)
        for j in range(2):
            proj_tm(("vr", j), lambda tti, ps, j=j: evac_copy(U[:, U_VR + 2 * tti + j, :], ps))
        def post_gr(m, ps, j):
            th = tmp_half()
            act(th, ps, AF.Tanh, scale=0.5)
            stt("dve", U[:, U_GR + 4 * j + m, :], th, 1.0, ps, ALU.add, ALU.mult)
        for j in range(2):
            proj_fm(("gr", j), None, lambda m, ps, j=j: post_gr(m, ps, j))
        for tti in range(4):
            b = next_bank()
            for h in range(4):
                tr(banks[b][1][:, h, :], U[:, U_KR + h, tti * 128:(tti + 1) * 128])
            tt("dve", U[:, U_KRT + tti, :].with_ap(lambda ap: ap.rearrange("p (h n) -> p h n", n=128)),
               banks[b][1][:, 0:4, :], cdec.full().with_ap(lambda ap: ap.unsqueeze(2).broadcast_to([128, 4, 128])), ALU.mult)

        if stop <= 2:
            continue
        for h in range(NH):
            W = SUBW[h]
            slope = SLOPES[h]
            acc = []
            for _ in range(2):
                b = next_bank()
                held.add(b)
                acc.append(b)
            O = [banks[acc[0]][0], banks[acc[1]][0]]
            den = [tmp_half(), tmp_half()]
            nkt = qi * 4 + 4
            jlist = []
            for j in range(nkt):
                k0 = j * 128
                mind = max(0, s0 - (k0 + 127))
                if slope * mind > CUT:
                    continue
                jlist.append(j)
            staged = {}

            def stage(j):
                k0 = j * 128
                r = j - qi * 4
                a_lo = max(0, r) * 128
                Sb = []
                for c in range(2):
                    b = next_bank()
                    S = banks[b][0]
                    mm(S[:, a_lo:QB], KT[64 * c:64 * c + 64, h, k0:k0 + 128], U[64 * c:64 * c + 64, U_QT + h, a_lo:QB], True, True)
                    Sb.append(S)
                pts = []
                for c in range(2):
                    S = Sb[c]
                    pt = PT[cnt["pt"] % 4]
                    cnt["pt"] += 1
                    for sb0 in range(0, QB, W):
                        lo = max(sb0, a_lo)
                        hi = sb0 + W
                        if lo >= hi:
                            continue
                        delta = s0 + sb0 - k0
                        act(pt[:, lo:hi], S[:, lo:hi], AF.Exp, bias=bias_col(h, delta))
                    if r >= 0:
                        dsl = pt[:, a_lo:a_lo + 128]
                        P.op("pool", lambda e, dsl=dsl: e.affine_select(out=dsl.ap, in_=dsl.ap, pattern=[[1, 128]],
                                                                        compare_op=ALU.is_ge, fill=0.0, base=0, channel_multiplier=-1),
                             reads=[dsl], writes=[dsl])
                    pts.append(pt)
                staged[j] = (pts, a_lo)

            def consume(j):
                pts, a_lo = staged.pop(j)
                st = (j == jlist[0])
                last = (j == jlist[-1])
                assert a_lo == 0 or not st
                for c in range(2):
                    mm(O[c][:, a_lo:QB], VC[:, j, h * 128:(h + 1) * 128], pts[c][:, a_lo:QB], st, last)
                for c in range(2):
                    if st:
                        cp("dve", den[c], pts[c].full())
                    else:
                        tt("dve", den[c].cols(a_lo, QB), den[c].cols(a_lo, QB), pts[c][:, a_lo:QB], ALU.add)

            n_it = len(jlist)
            stage(jlist[0])
            run_deferred(1)
            for i in range(n_it):
                if i + 1 < n_it:
                    stage(jlist[i + 1])
                consume(jlist[i])
                if i == min(3, n_it - 1):
                    run_deferred(2)

            def part_a(O=O, den=den, acc=acc):
                for c in range(2):
                    b = next_bank()
                    mm(banks[b][0].full(), ones_f.full(), den[c], True, True)
                    act(den[c], banks[b][0].full(), AF.Ln, scale=2.0 ** -8)
                    act(den[c], den[c], AF.Exp, scale=-1.0, bias=lnk)
                tt("dve", den[0], O[0].full(), den[0], ALU.mult)
                tt("dve", den[1], O[1].full(), den[1], ALU.mult)
                for b in acc:
                    held.discard(b)
                stt("dve", yraw_d.full(), den[1], neg_lam, den[0], ALU.mult, ALU.add)

            def part_b(h=h):
                act(sqb_d.full(), yraw_d.full(), AF.Square)
                b = next_bank()
                mm(banks[b][0].full(), ones.full(), sqb_d.full(), True, True)
                rstd = tmp_half()
                act(rstd, banks[b][0].full(), AF.Ln, scale=1.0 / 128, bias=epsc[:, 0:1])
                act(rstd, rstd, AF.Exp, scale=-0.5)
                stt("dve", U[:, U_YA + h, :], yraw_d.full(), gdcol.full(), rstd, ALU.mult, ALU.mult)
            run_deferred(2)
            deferred.append([part_a, part_b])
            run_deferred(1)

        if stop <= 3:
            continue
        def ret_scores(tti):
            tsl = slice(tti * 128, (tti + 1) * 128)
            b = next_bank()
            for h in range(4):
                mm(banks[b][0][:, h * 128:(h + 1) * 128], U[:, U_KR + h, tsl], U[:, U_QR + h, tsl], True, True)
            sm = Sm[tti % 2]
            tt("dve", sm.full(), banks[b][0].full().with_ap(lambda ap: ap.rearrange("p (h n) -> p h n", n=128)),
               trimask.full().with_ap(lambda ap: ap.unsqueeze(1).broadcast_to([128, 4, 128])), ALU.mult)

        def ret_transposes(tti):
            tsl = slice(tti * 128, (tti + 1) * 128)
            yt = yrtok[tti % 2]
            b = next_bank()
            for k in range(8):
                tr(banks[b][1][:, k, :], yt[:, k * 128:(k + 1) * 128])
            tt("dve", U[:, U_YR:U_YR + 8, tsl], banks[b][1].full(), U[:, U_GR:U_GR + 8, tsl], ALU.mult)

        ret_scores(0)
        for tti in range(4):
            tsl = slice(tti * 128, (tti + 1) * 128)
            rb_ = [next_bank(), next_bank()]
            for h in range(4):
                o = banks[rb_[h // 2]][0][:, (h % 2) * 256:(h % 2 + 1) * 256]
                vr = U[:, U_VR + 2 * tti + h // 2, (h % 2) * 256:(h % 2 + 1) * 256]
                mm(o, U[:, U_KRT + tti, h * 128:(h + 1) * 128], vr, True, True)
            if tti < 3:
                ret_scores(tti + 1)
            if tti == 0:
                run_deferred(1)
            if tti == 1:
                run_deferred(2)
            sm = Sm[tti % 2]
            ob = [next_bank(), next_bank()]
            for h in range(4):
                o = banks[ob[h // 2]][0][:, (h % 2) * 256:(h % 2 + 1) * 256]
                vr = U[:, U_VR + 2 * tti + h // 2, (h % 2) * 256:(h % 2 + 1) * 256]
                mm(o, sm[:, h, :], vr, True, False)
                mm(o, U[:, U_QR + h, tsl], Rb[:, h, :], False, True)
            if tti > 0:
                ret_transposes(tti - 1)
            for h in range(4):
                o = banks[rb_[h // 2]][0][:, (h % 2) * 256:(h % 2 + 1) * 256]
                stt("dve", Rf[:, h, :], Rf[:, h, :], GAM[h] ** 128, o, ALU.mult, ALU.add)
            cp("dve", Rb.full(), Rf.full())
            yt = yrtok[tti % 2]
            sc = 4 * (tti % 2)
            junk = hb[tti % 2]
            for h in range(4):
                o = banks[ob[h // 2]][0][:, (h % 2) * 256:(h % 2 + 1) * 256]
                act(junk[:, h * 256:(h + 1) * 256], o, AF.Square, accum=stat[:, sc + h:sc + h + 1])
            rsqrt_cols(stat[:, sc + 8:sc + 12], stat[:, sc:sc + 4], 4, 1.0 / 256)
            for h in range(4):
                o = banks[ob[h // 2]][0][:, (h % 2) * 256:(h % 2 + 1) * 256]
                stt("dve", yt[:, h * 256:(h + 1) * 256], o, stat[:, sc + 8 + h:sc + 9 + h], gret_b[:, h * 256:(h + 1) * 256], ALU.mult, ALU.mult)
        ret_transposes(3)

        if stop <= 4:
            continue
        for tti in range(4):
            P.dma("sp", xres[:, tti, :], x[t0 + tti * 128: t0 + (tti + 1) * 128, :])

        for j in range(2):
            wt = wget(("ga", j))
            for m in range(4):
                b = next_bank()
                for k in range(8):
                    mm(banks[b][0].full(), wt[:, k, m * 128:(m + 1) * 128], hT[:, k, :], k == 0, k == 7)
                act(U[:, U_TA + m, :], banks[b][0].full(), AF.Tanh, scale=0.5)
            wt = wget(("wd", j))
            for m in range(4):
                b = next_bank()
                for k in range(8):
                    mm(banks[b][0].full(), wt[:, k, m * 128:(m + 1) * 128], U[:, U_YA + k, :], k == 0, k == 7)
                stt("dve", U[:, U_MIX + 4 * j + m, :], U[:, U_TA + m, :], 1.0, banks[b][0].full(), ALU.add, ALU.mult)
            wt = wget(("gb", j))
            for m in range(4):
                b = next_bank()
                for k in range(8):
                    mm(banks[b][0].full(), wt[:, k, m * 128:(m + 1) * 128], hT[:, k, :], k == 0, k == 7)
                act(U[:, U_TA + m, :], banks[b][0].full(), AF.Tanh, scale=0.5)
            wt = wget(("wr", j))
            for m in range(4):
                b = next_bank()
                for k in range(8):
                    mm(banks[b][0].full(), wt[:, k, m * 128:(m + 1) * 128], U[:, U_YR + k, :], k == 0, k == 7)
                th = tmp_half()
                stt("dve", th, U[:, U_TA + m, :], 1.0, banks[b][0].full(), ALU.add, ALU.mult)
                tt("dve", U[:, U_MIX + 4 * j + m, :], U[:, U_MIX + 4 * j + m, :], th, ALU.add)

        if stop <= 5:
            continue
        wts = [wget(("wo", 0)), wget(("wo", 1), ahead=0)]
        for tti in range(4):
            for n in range(2):
                wt = wts[n]
                b = next_bank()
                for k in range(8):
                    mm(banks[b][0].full(), U[:, U_MIX + k, tti * 128:(tti + 1) * 128], wt[:, k, :], k == 0, k == 7)
                xs = xres[:, tti, n * 512:(n + 1) * 512]
                stt("dve", xs, banks[b][0].full(), 0.5, xs, ALU.mult, ALU.add)
            if tti > 1:
                rms_to_hT(xres[:, tti - 2, :], tti - 2, 1, (tti - 2) % 2)
        rms_to_hT(xres[:, 2, :], 2, 1, 0)
        rms_to_hT(xres[:, 3, :], 3, 1, 1)

        if stop <= 7:
            continue

        if stop <= 7:
            continue
        for j in range(8):
            wt = wget(("f1", j))
            for m in range(4):
                b = next_bank()
                for k in range(8):
                    mm(banks[b][0].full(), wt[:, k, m * 128:(m + 1) * 128], hT[:, k, :], k == 0, k == 7)
                th = tmp_half()
                act(th, banks[b][0].full(), AF.Square)
                stt("dve", U[:, 4 * j + m, :], banks[b][0].full(), 0.0, th, ALU.is_gt, ALU.mult)

        if stop <= 8:
            continue
        for n in range(2):
            accb = []
            for _ in range(4):
                b = next_bank()
                held.add(b)
                accb.append(b)
            for g in range(4):
                wt = wget(("f2", n * 4 + g))
                for tti in range(4):
                    for k in range(8):
                        mm(banks[accb[tti]][0].full(), U[:, g * 8 + k, tti * 128:(tti + 1) * 128], wt[:, k, :],
                           g == 0 and k == 0, g == 3 and k == 7)
            for tti in range(4):
                xs = xres[:, tti, n * 512:(n + 1) * 512]
                tt("dve", xs, banks[accb[tti]][0].full(), xs, ALU.add)
                held.discard(accb[tti])

        if stop <= 9:
            continue
        for tti in range(4):
            rms_to_hT(xres[:, tti, :], tti, 2, tti % 2)
        wt = wget(("ple", 0))
        for tti in range(4):
            pi = pinb[tti % 2]
            P.dma("sp", pi.full(), pin[t0 + tti * 128: t0 + (tti + 1) * 128, :])
            cp("pool", pb16.full(), pi.full())
            b = next_bank()
            for kc in range(2):
                tr(banks[b][1][:, kc, :], pb16[:, kc * 128:(kc + 1) * 128])
            cp("dve", ppT[:, :, tti * 128:(tti + 1) * 128], banks[b][1][:, 0:2, :])
            for n in range(2):
                b = next_bank()
                for kc in range(2):
                    mm(banks[b][0].full(), ppT[:, kc, tti * 128:(tti + 1) * 128], wt[:, 2 * n + kc, :], kc == 0, kc == 1)
                evac_copy(U[:, U_YA + 2 * tti + n, :], banks[b][0].full())
        for n in range(2):
            wt = wget(("pg", n))
            for tti in range(4):
                b = next_bank()
                for k in range(8):
                    mm(banks[b][0].full(), hT[:, k, tti * 128:(tti + 1) * 128], wt[:, k, :], k == 0, k == 7)
                th = tmp_half()
                act(th, banks[b][0].full(), AF.Tanh, scale=0.5)
                stt("dve", th, th, 1.0, U[:, U_YA + 2 * tti + n, :], ALU.add, ALU.mult)
                xs = xres[:, tti, n * 512:(n + 1) * 512]
                stt("dve", xs, th, 0.5, xs, ALU.mult, ALU.add)
        for tti in range(4):
            sc = tti % 2
            junk = hb[sc]
            act(junk.full(), xres[:, tti, :], AF.Square, accum=stat[:, sc:sc + 1])
            rsqrt_cols(stat[:, sc + 8:sc + 9], stat[:, sc:sc + 1], 1, 1.0 / D)
            ot = tmp_full()
            stt("dve", ot.full(), xres[:, tti, :], stat[:, sc + 8:sc + 9], gfin_b.full(), ALU.mult, ALU.mult)
            P.dma("pool", out[t0 + tti * 128: t0 + (tti + 1) * 128, :], ot.full())

    P.barrier("pool", [out.full()])
    P.barrier("sp", [out.full()])
    P.emit()
    P.close()
    return nc, P


_CACHE = {}


def kernel(**inputs):
    n = 8
    xs = np.ascontiguousarray(np.asarray(inputs["x"], dtype=np.float32)).reshape(n, TOK, D)
    ps = np.ascontiguousarray(np.asarray(inputs["p"], dtype=np.float32)).reshape(n, TOK, PLE)
    shared = {}
    for k in ("g_mix", "g_mlp", "g_ple", "w_in", "w_branch_diff", "w_branch_ret", "w_out", "w_ff1", "w_ff2",
              "w_ple_gate", "w_ple", "lam_q1", "lam_k1", "lam_q2", "lam_k2", "g_diff_sub"):
        a = np.asarray(inputs[k], dtype=np.float32)
        shared[k] = np.ascontiguousarray(a.reshape(a.shape[1:]))
    shared["g_ret_sub"] = np.ascontiguousarray(np.asarray(inputs["g_ret_sub"], dtype=np.float32).reshape(1024))
    shared["g_final"] = np.ascontiguousarray(np.asarray(inputs["g_final"], dtype=np.float32))
    if "nc" not in _CACHE:
        _CACHE["nc"] = build_program()[0]
    nc = _CACHE["nc"]
    in_maps = [dict(shared, x=xs[i], p=ps[i]) for i in range(n)]
    res = run_bass_kernel_spmd(nc, in_maps, core_ids=list(range(n)))
    outs = [np.asarray(res.results[i]["out"], dtype=np.float32).reshape(NSEQ, SEQ, D) for i in range(n)]
    return np.concatenate(outs, axis=0)
```

```python
import contextlib
import math
import numpy as np
import concourse.bass as bass
import concourse.mybir as mybir
from concourse.bass_utils import run_bass_kernel_spmd

F32 = mybir.dt.float32
BF16 = mybir.dt.bfloat16
AF = mybir.ActivationFunctionType
ALU = mybir.AluOpType
AX = mybir.AxisListType


def _norm_idx(idx, shape):
    if not isinstance(idx, tuple):
        idx = (idx,)
    box = []
    for d, n in enumerate(shape):
        if d < len(idx):
            i = idx[d]
            if isinstance(i, slice):
                lo = 0 if i.start is None else i.start
                hi = n if i.stop is None else i.stop
            else:
                lo, hi = int(i), int(i) + 1
        else:
            lo, hi = 0, n
        assert 0 <= lo < hi <= n, (idx, shape)
        box.append((lo, hi))
    return tuple(box)


class View:
    __slots__ = ("buf", "ap", "box")

    def __init__(self, buf, ap, box):
        self.buf, self.ap, self.box = buf, ap, box

    def with_ap(self, fn):
        return View(self.buf, fn(self.ap), self.box)

    def cols(self, lo, hi):
        b0 = self.box[-1][0]
        return View(self.buf, self.ap[:, lo:hi], self.box[:-1] + ((b0 + lo, b0 + hi),))


class Buf:
    def __init__(self, prog, name, handle, shape, tracked=True, whole=False):
        self.prog, self.name, self.handle, self.shape = prog, name, handle, tuple(shape)
        self.tracked = tracked
        self.whole = whole
        self.group = None
        self.hist = []

    def __getitem__(self, idx):
        box = _norm_idx(idx, self.shape)
        if self.whole:
            box = tuple((0, n) for n in self.shape)
        return View(self, self.handle[idx], box)

    def full(self):
        return self[tuple(slice(None) for _ in self.shape)]


def _overlap(a, b):
    for (l0, h0), (l1, h1) in zip(a, b):
        if h0 <= l1 or h1 <= l0:
            return False
    return True


def _contains(outer, inner):
    for (l0, h0), (l1, h1) in zip(outer, inner):
        if l1 < l0 or h1 > h0:
            return False
    return True


class Instr:
    __slots__ = ("eng", "fn", "clock", "pos", "waits", "signal", "is_dma", "snap")


ENGS = ("pe", "act", "dve", "pool", "sp")
DMA_SLOTS = {"sp": 8, "act": 4, "pool": 8}


class Prog:
    def __init__(self, nc):
        self.nc = nc
        self.stack = contextlib.ExitStack()
        self.instrs = {e: [] for e in ENGS}
        self.known = {e: {} for e in ENGS}
        self.clock_pos = {}
        self.clock_instrs = {}
        self.dma_n = {e: 0 for e in DMA_SLOTS}
        self.n_total = 0

    def sbuf(self, name, shape, dtype):
        h = self.stack.enter_context(self.nc.sbuf_tensor(name, list(shape), dtype))
        return Buf(self, name, h, shape)

    def psum(self, name, shape, dtype=F32):
        h = self.stack.enter_context(self.nc.psum_tensor(name, list(shape), dtype))
        return Buf(self, name, h, shape, whole=True)

    def dram(self, name, shape, dtype, kind="Internal", tracked=True):
        h = self.nc.dram_tensor(name, list(shape), dtype, kind=kind)
        return Buf(self, name, h, shape, tracked=tracked)

    def alias(self, name, base, new_handle, shape, whole=False):
        b = Buf(self, name, new_handle, shape, whole=whole)
        if base.group is None:
            base.group = [base]
        base.group.append(b)
        b.group = base.group
        return b

    def _deps_for(self, view, is_write, deps, eng):
        buf = view.buf
        if not buf.tracked:
            return
        bufs = buf.group if buf.group is not None else (buf,)
        for b in bufs:
            same = b is buf
            for ebox, ewrite, eclock, epos in b.hist:
                if not (is_write or ewrite):
                    continue
                if same and not _overlap(ebox, view.box):
                    continue
                if eclock == "pe" and eng == "pe":
                    continue
                if deps.get(eclock, 0) < epos:
                    deps[eclock] = epos

    def _record(self, view, is_write, clock, pos, in_order):
        buf = view.buf
        if not buf.tracked:
            return
        box = view.box
        h = buf.hist
        if is_write:
            h[:] = [e for e in h if not _contains(box, e[0])]
            if buf.group is not None and buf.whole:
                for b in buf.group:
                    if b is not buf:
                        b.hist[:] = []
        elif in_order:
            h[:] = [e for e in h if not (e[2] == clock and not e[1] and _contains(box, e[0]))]
        h.append((box, is_write, clock, pos))

    def op(self, eng, fn, reads=(), writes=(), dma=False):
        ins = Instr()
        ins.eng, ins.fn, ins.is_dma, ins.signal = eng, fn, dma, dma
        if dma:
            n = self.dma_n[eng]
            self.dma_n[eng] = n + 1
            clock = ("dma", eng, n % DMA_SLOTS[eng])
        else:
            clock = eng
        pos = self.clock_pos.get(clock, 0) + 1
        self.clock_pos[clock] = pos
        self.clock_instrs.setdefault(clock, []).append(ins)
        ins.clock, ins.pos = clock, pos
        deps = {}
        if dma and pos > 1:
            deps[clock] = pos - 1
        for v in reads:
            self._deps_for(v, False, deps, eng)
        for v in writes:
            self._deps_for(v, True, deps, eng)
        known = self.known[eng]
        waits = []
        for ck, p in deps.items():
            if known.get(ck, 0) >= p:
                continue
            waits.append((ck, p))
            j = self.clock_instrs[ck][p - 1]
            j.signal = True
            for k2, p2 in j.snap.items():
                if known.get(k2, 0) < p2:
                    known[k2] = p2
            known[ck] = p
        ins.waits = waits
        ins.snap = dict(known)
        if fn is not None:
            for v in reads:
                self._record(v, False, clock, pos, not dma)
            for v in writes:
                self._record(v, True, clock, pos, not dma)
        self.instrs[eng].append(ins)
        self.n_total += 1
        return ins

    def dma(self, queue, out, in_, **kw):
        def fn(e):
            return e.dma_start(out=out.ap, in_=in_.ap, **kw)
        return self.op(queue, fn, reads=[in_], writes=[out], dma=True)

    def barrier(self, eng, views):
        return self.op(eng, None, reads=(), writes=views)

    def emit(self):
        nc = self.nc
        SEM_EPOCH = 1024
        sems = {}
        val = {}
        semof = {}
        for ck, lst in self.clock_instrs.items():
            nm = ck if isinstance(ck, str) else "d_%s_%d" % (ck[1], ck[2])
            step = 1 if isinstance(ck, str) else 16
            c = 0
            for ins in lst:
                if ins.signal:
                    ep, r = divmod(c, SEM_EPOCH // step)
                    c += 1
                    if (ck, ep) not in sems:
                        sems[(ck, ep)] = self.stack.enter_context(nc.semaphore("s_%s_%d" % (nm, ep)))
                    val[(ck, ins.pos)] = (r + 1) * step
                    semof[(ck, ins.pos)] = sems[(ck, ep)]
        engmap = {"pe": "tensor", "act": "scalar", "dve": "vector", "pool": "gpsimd", "sp": "sync"}
        with nc.Block() as block:
            for ename in ENGS:
                lst = self.instrs[ename]

                def body(e, lst=lst):
                    for ins in lst:
                        for ck, p in ins.waits:
                            e.wait_ge(semof[(ck, p)], val[(ck, p)])
                        if ins.fn is None:
                            continue
                        r = ins.fn(e)
                        if ins.signal:
                            r.then_inc(semof[(ins.clock, ins.pos)], 16 if ins.is_dma else 1)

                getattr(block, engmap[ename])(body)

    def close(self):
        self.stack.close()


D = 1024
SEQ = 2048
NSEQ = 4
TOK = NSEQ * SEQ
QB = 512
NQB_SEQ = SEQ // QB
PLE = 256
DFF = 4096
EPS = 1e-6
LAM_INIT = 0.8 - 0.6 * math.exp(-0.3 * 0)
NH = 8
SLOPES = [2.0 ** (-(h + 1)) for h in range(NH)]
SUBW = [128, 256, 512, 512, 512, 512, 512, 512]
GAM = [1.0 - 2.0 ** (-5.0 - h) for h in range(4)]
LNG = [math.log(g) for g in GAM]
CUT = 60.0

WT = {}
_t = 0
for nm, c0 in (("qa", 0), ("ka", 1024), ("va", 2048)):
    for j in range(2):
        WT[(nm, j)] = (_t, "w_in", 0, c0 + 512 * j); _t += 1
WT[("qr", 0)] = (_t, "w_in", 0, 3072); _t += 1
WT[("kr", 0)] = (_t, "w_in", 0, 3584); _t += 1
for nm, c0 in (("vr", 4096), ("gr", 5120), ("ga", 6144), ("gb", 7168)):
    for j in range(2):
        WT[(nm, j)] = (_t, "w_in", 0, c0 + 512 * j); _t += 1
for nm, src in (("wd", "w_branch_diff"), ("wr", "w_branch_ret"), ("wo", "w_out")):
    for j in range(2):
        WT[(nm, j)] = (_t, src, 0, 512 * j); _t += 1
for j in range(8):
    WT[("f1", j)] = (_t, "w_ff1", 0, 512 * j); _t += 1
for n in range(2):
    for g in range(4):
        WT[("f2", n * 4 + g)] = (_t, "w_ff2", 1024 * g, 512 * n); _t += 1
for j in range(2):
    WT[("pg", j)] = (_t, "w_ple_gate", 0, 512 * j); _t += 1
WT[("ple", 0)] = (_t, "w_ple", 0, 0); _t += 1
NWT = _t

QB_SCHED = ([("qa", 0), ("qa", 1), ("ka", 0), ("ka", 1), ("va", 0), ("va", 1), ("qr", 0), ("kr", 0),
             ("vr", 0), ("vr", 1), ("gr", 0), ("gr", 1)]
            + [("ga", 0), ("wd", 0), ("gb", 0), ("wr", 0), ("ga", 1), ("wd", 1), ("gb", 1), ("wr", 1)]
            + [("wo", 0), ("wo", 1)]
            + [("f1", j) for j in range(8)]
            + [("f2", j) for j in range(8)]
            + [("ple", 0), ("pg", 0), ("pg", 1)])


def build_program(n_qb=16, nslots=2, stop=99):
    nc = bass.Bass("TRN2", target_bir_lowering=False)
    P = Prog(nc)
    ext = lambda n, s: P.dram(n, s, F32, kind="ExternalInput", tracked=False)
    x = ext("x", [TOK, D])
    pin = ext("p", [TOK, PLE])
    g_mix = ext("g_mix", [D]); g_mlp = ext("g_mlp", [D]); g_ple = ext("g_ple", [D]); g_final = ext("g_final", [D])
    wsrc = {"w_in": ext("w_in", [D, 8192]), "w_branch_diff": ext("w_branch_diff", [D, D]),
            "w_branch_ret": ext("w_branch_ret", [D, D]), "w_out": ext("w_out", [D, D]),
            "w_ff1": ext("w_ff1", [D, DFF]), "w_ff2": ext("w_ff2", [DFF, D]),
            "w_ple_gate": ext("w_ple_gate", [D, D]), "w_ple": ext("w_ple", [PLE, D])}
    lam_in = [ext(n, [64]) for n in ("lam_q1", "lam_k1", "lam_q2", "lam_k2")]
    g_diff_sub = ext("g_diff_sub", [128])
    g_ret_sub = ext("g_ret_sub", [1024])
    out = P.dram("out", [TOK, D], F32, kind="ExternalOutput")
    wb = P.dram("wb", [NWT, 128, 8, 512], BF16)

    KT = P.sbuf("KT", [128, NH, SEQ], BF16)
    VC = P.sbuf("VC", [128, SEQ // 128, D], BF16)
    wsl = [P.sbuf("wsl%d" % i, [128, 8, 512], BF16) for i in range(nslots)]
    xres = P.sbuf("xres", [128, 4, D], F32)
    hT = P.sbuf("hT", [128, 8, QB], BF16)
    U = P.sbuf("U", [128, 52, QB], BF16)
    QT = lambda h: U[:, h, :]
    tmpf = [P.sbuf("tmpf%d" % i, [128, D], F32) for i in range(3)]
    PT = [P.sbuf("PT%d" % i, [128, QB], BF16) for i in range(4)]
    hb = [P.sbuf("hb%d" % i, [128, D], BF16) for i in range(1)] * 2
    ident = P.sbuf("ident", [128, 128], BF16)
    ones = P.sbuf("ones", [128, 128], BF16)
    iota_p = P.sbuf("iota_p", [128, 1], F32)
    mhalf = P.sbuf("mhalf", [128, 4], F32)
    bias_tab = P.sbuf("bias_tab", [128, NH * 24], F32)
    trimask = P.sbuf("trimask", [128, 128], BF16)
    qdec = P.sbuf("qdec", [128, 4, 128], F32)
    kdec = P.sbuf("kdec", [128, 4, 128], F32)
    cdec = P.sbuf("cdec", [128, 4], F32)
    gcol = P.sbuf("gcol", [128, 3, 8], F32)
    gdcol = P.sbuf("gdcol", [128, 1], F32)
    gret_b = P.sbuf("gret_b", [128, D], F32)
    gfin_b = P.sbuf("gfin_b", [128, D], F32)
    lamt = P.sbuf("lamt", [128, 4, 64], F32)
    lams = P.sbuf("lams", [128, 8], F32)
    stat = P.sbuf("stat", [128, 16], F32)
    Rf = P.sbuf("Rf", [128, 4, 256], F32)
    Rb = P.sbuf("Rb", [128, 4, 256], BF16)
    Sm = [P.sbuf("Sm%d" % i, [128, 4, 128], BF16) for i in range(2)]
    yrtok = [P.sbuf("yrtok%d" % i, [128, D], BF16) for i in range(2)]
    pinb = [P.sbuf("pinb%d" % i, [128, PLE], F32) for i in range(1)] * 2
    pb16 = P.sbuf("pb16", [128, PLE], BF16)
    ppT = P.sbuf("ppT", [128, 2, QB], BF16)

    banks = []
    for i in range(8):
        bf = P.psum("pb%d" % i, [128, 512], F32)
        bh = P.alias("pbh%d" % i, bf, bf.handle.bitcast(BF16).reshape([128, 8, 128]), [128, 8, 128], whole=True)
        banks.append((bf, bh))
    held = set()
    bank_ptr = [0]

    def next_bank():
        while True:
            i = bank_ptr[0] % 8
            bank_ptr[0] += 1
            if i not in held:
                return i

    def ap_of(v):
        return v.ap if isinstance(v, View) else v

    def vlist(*vs):
        return [v for v in vs if isinstance(v, View)]

    def mm(o, lhsT, rhs, start, stop):
        P.op("pe", lambda e: e.matmul(o.ap, lhsT=lhsT.ap, rhs=rhs.ap, start=start, stop=stop),
             reads=[lhsT, rhs], writes=[o])

    idv = ident.full()

    def tr(o, i_):
        P.op("pe", lambda e: e.transpose(out=o.ap, in_=i_.ap, identity=idv.ap), reads=[i_, idv], writes=[o])

    def act(o, i_, func, scale=1.0, bias=None, accum=None):
        kw = {}
        if bias is not None:
            kw["bias"] = ap_of(bias)
        if accum is not None:
            kw["accum_out"] = accum.ap
        P.op("act", lambda e: e.activation(out=o.ap, in_=i_.ap, func=func, scale=ap_of(scale), **kw),
             reads=vlist(i_, scale, bias), writes=vlist(o, accum))

    def tt(eng, o, a, b, op):
        P.op(eng, lambda e: e.tensor_tensor(out=o.ap, in0=a.ap, in1=b.ap, op=op), reads=[a, b], writes=[o])

    def ts(eng, o, a, s1, s2, op0, op1=None):
        if op1 is None:
            P.op(eng, lambda e: e.tensor_scalar(out=o.ap, in0=a.ap, scalar1=ap_of(s1), scalar2=None, op0=op0),
                 reads=vlist(a, s1), writes=[o])
        else:
            P.op(eng, lambda e: e.tensor_scalar(out=o.ap, in0=a.ap, scalar1=ap_of(s1), scalar2=ap_of(s2), op0=op0, op1=op1),
                 reads=vlist(a, s1, s2), writes=[o])

    def stt(eng, o, a, s, b, op0, op1):
        P.op(eng, lambda e: e.scalar_tensor_tensor(out=o.ap, in0=a.ap, scalar=ap_of(s), in1=b.ap, op0=op0, op1=op1),
             reads=vlist(a, s, b), writes=[o])

    def cp(eng, o, a):
        P.op(eng, lambda e: e.tensor_copy(out=o.ap, in_=a.ap), reads=[a], writes=[o])

    def memset(eng, o, val):
        P.op(eng, lambda e: e.memset(o.ap, val), writes=[o])

    def bcast(v, shape):
        return v.with_ap(lambda ap: ap.broadcast_to(shape))

    epsc = P.sbuf("epsc", [128, 1], F32)
    memset("pool", epsc.full(), EPS)
    memset("pool", tmpf[0][:, 0:128], 1.0)
    P.op("pool", lambda e: e.affine_select(out=tmpf[0][:, 0:128].ap, in_=tmpf[0][:, 0:128].ap, pattern=[[-1, 128]],
                                           compare_op=ALU.is_equal, fill=0.0, base=0, channel_multiplier=1),
         reads=[tmpf[0][:, 0:128]], writes=[tmpf[0][:, 0:128]])
    cp("dve", ident.full(), tmpf[0][:, 0:128])
    memset("pool", tmpf[0][:, 0:128], 1.0)
    cp("dve", ones.full(), tmpf[0][:, 0:128])
    P.op("pool", lambda e: e.affine_select(out=tmpf[0][:, 0:128].ap, in_=tmpf[0][:, 0:128].ap, pattern=[[1, 128]],
                                           compare_op=ALU.is_ge, fill=0.0, base=0, channel_multiplier=-1),
         reads=[tmpf[0][:, 0:128]], writes=[tmpf[0][:, 0:128]])
    cp("dve", trimask.full(), tmpf[0][:, 0:128])
    P.op("pool", lambda e: e.iota(iota_p.full().ap, pattern=[[0, 1]], base=0, channel_multiplier=1,
                                  allow_small_or_imprecise_dtypes=True), writes=[iota_p.full()])
    P.op("pool", lambda e: e.iota(tmpf[1][:, 0:128].ap, pattern=[[1, 128]], base=1, channel_multiplier=0,
                                  allow_small_or_imprecise_dtypes=True), writes=[tmpf[1][:, 0:128]])
    def bias_col(h, delta):
        di = delta // 128 + 3
        assert 0 <= di < 24
        return bias_tab[:, h * 24 + di: h * 24 + di + 1]
    for h in range(NH):
        for di in range(24):
            delta = 128 * (di - 3)
            ts("dve", bias_tab[:, h * 24 + di: h * 24 + di + 1], iota_p.full(), SLOPES[h],
               -SLOPES[h] * (delta + SUBW[h] // 2), ALU.mult, ALU.add)
    for h in range(4):
        act(qdec[:, h, :], tmpf[1][:, 0:128], AF.Exp, scale=LNG[h])
        act(kdec[:, h, :], tmpf[1][:, 0:128], AF.Exp, scale=-LNG[h], bias=None)
        ts("dve", kdec[:, h, :], kdec[:, h, :], 128.0 ** -0.5, None, ALU.mult)
        memset("pool", cdec[:, h:h + 1], GAM[h] ** 128)
    for i, gsrc in enumerate((g_mix, g_mlp, g_ple)):
        src = View(gsrc, gsrc.handle.ap().rearrange("(c p) -> p c", p=128), ((0, D),))
        P.dma("sp", gcol[:, i, :], src, allow_slow_non_contiguous=True)
    P.dma("sp", gdcol.full(), View(g_diff_sub, g_diff_sub.handle.ap().rearrange("(p o) -> p o", o=1), ((0, 128),)),
          allow_slow_non_contiguous=True)
    ts("dve", gdcol.full(), gdcol.full(), 1.0 - LAM_INIT, None, ALU.mult)
    P.dma("sp", gret_b.full(), View(g_ret_sub, g_ret_sub.handle.ap().partition_broadcast(128), ((0, D),)))
    ts("dve", gret_b.full(), gret_b.full(), 0.5, None, ALU.mult)
    P.dma("sp", gfin_b.full(), View(g_final, g_final.handle.ap().partition_broadcast(128), ((0, D),)))
    for i, lsrc in enumerate(lam_in):
        P.dma("sp", lamt[:, i, :], View(lsrc, lsrc.handle.ap().partition_broadcast(128), ((0, 64),)))
    tt("dve", lamt[:, 0, :], lamt[:, 0, :], lamt[:, 1, :], ALU.mult)
    tt("dve", lamt[:, 2, :], lamt[:, 2, :], lamt[:, 3, :], ALU.mult)
    P.op("dve", lambda e: e.reduce_sum(out=lams[:, 0:1].ap, in_=lamt[:, 0, :].ap, axis=AX.X), reads=[lamt[:, 0, :]], writes=[lams[:, 0:1]])
    P.op("dve", lambda e: e.reduce_sum(out=lams[:, 1:2].ap, in_=lamt[:, 2, :].ap, axis=AX.X), reads=[lamt[:, 2, :]], writes=[lams[:, 1:2]])
    act(lams[:, 2:4], lams[:, 0:2], AF.Exp)
    tt("dve", lams[:, 4:5], lams[:, 3:4], lams[:, 2:3], ALU.subtract)
    ts("dve", lams[:, 4:5], lams[:, 4:5], -LAM_INIT, None, ALU.add)
    neg_lam = lams[:, 4:5]

    conv_order = []
    for key in QB_SCHED:
        if key not in conv_order:
            conv_order.append(key)
    for key in conv_order:
        t, src, r0, c0 = WT[key]
        w = wsrc[src]
        if key[0] == "ple":
            for hf in range(2):
                sv = View(w, w.handle.ap()[:, hf * 512:(hf + 1) * 512].rearrange("(c p) n -> p c n", p=128), ((0, 1), (0, 1)))
                P.dma("pool", wb[t, :, 2 * hf:2 * hf + 2, :], sv)
        else:
            sv = View(w, w.handle.ap()[r0:r0 + 1024, c0:c0 + 512].rearrange("(c p) n -> p c n", p=128), ((0, 1), (0, 1)))
            P.dma("pool", wb[t], sv)

    sched = [WT[k][0] for _ in range(n_qb) for k in QB_SCHED]
    ws = {"issued": 0, "next": 0}

    def wget(expect_key, ahead=None):
        i = ws["next"]
        assert sched[i] == WT[expect_key][0], (expect_key, i)
        ahead = nslots - 1 if ahead is None else ahead
        while ws["issued"] < min(len(sched), i + 1 + ahead):
            j = ws["issued"]
            if sched[j] == WT[("ple", 0)][0]:
                P.dma("sp", wsl[j % nslots][:, 0:4, :], wb[sched[j], :, 0:4, :])
            else:
                P.dma("sp", wsl[j % nslots].full(), wb[sched[j]])
            ws["issued"] += 1
        ws["next"] += 1
        return wsl[i % nslots]

    cnt = {"evac": 0, "tmp": 0, "pt": 0}
    yraw_d = P.sbuf("yraw_d", [128, QB], F32)
    sqb_d = P.sbuf("sqb_d", [128, QB], BF16)
    ones_f = P.sbuf("ones_f", [128, 128], F32)
    memset("pool", ones_f.full(), 1.0)
    lnk = lams[:, 5:6]
    memset("pool", lnk, -8.0 * math.log(2.0))
    deferred = []

    def run_deferred(level):
        for d in list(deferred):
            if d[0] is not None:
                f = d[0]
                d[0] = None
                f()
            if level >= 2 and d[1] is not None:
                f = d[1]
                d[1] = None
                f()
            if d[0] is None and d[1] is None:
                deferred.remove(d)

    def tmp_half():
        i = cnt["tmp"] % 6
        cnt["tmp"] += 1
        return tmpf[i // 2][:, (i % 2) * 512:(i % 2 + 1) * 512]

    def tmp_full():
        i = (cnt["tmp"] + 1) // 2 % 3
        cnt["tmp"] = (i + 1) * 2
        return tmpf[i]

    def evac_copy(o, i_, scale=None):
        cnt["evac"] += 1
        if cnt["evac"] % 2:
            act(o, i_, AF.Copy, scale=1.0 if scale is None else scale)
        elif scale is None:
            cp("dve", o, i_)
        else:
            ts("dve", o, i_, scale, None, ALU.mult)

    def rsqrt_cols(o, ssq, n, inv_n):
        act(o, ssq, AF.Ln, scale=inv_n, bias=epsc[:, 0:1])
        act(o, o, AF.Exp, scale=-0.5)

    def rms_to_hT(src_tile, tcol, gi, scol):
        hbuf = hb[scol % 2]
        act(hbuf.full(), src_tile, AF.Square, accum=stat[:, scol:scol + 1])
        rsqrt_cols(stat[:, scol + 8:scol + 9], stat[:, scol:scol + 1], 1, 1.0 / D)
        act(hbuf.full(), src_tile, AF.Copy, scale=stat[:, scol + 8:scol + 9])
        b = next_bank()
        for k in range(8):
            tr(banks[b][1][:, k, :], hbuf[:, k * 128:(k + 1) * 128])
        tt("dve", hT[:, :, tcol * 128:(tcol + 1) * 128], banks[b][1].full(),
           gcol[:, gi, :].with_ap(lambda ap: ap.unsqueeze(2).broadcast_to([128, 8, 128])), ALU.mult)

    U_QT, U_QR, U_KR, U_KRT, U_VR, U_YA, U_YR, U_GR = 0, 8, 12, 16, 20, 28, 36, 44
    U_MIX, U_TA = 0, 8

    for qb in range(n_qb):
        sq = qb // NQB_SEQ
        qi = qb % NQB_SEQ
        s0 = qi * QB
        t0 = sq * SEQ + s0

        for tti in range(4):
            xt = tmp_full()
            P.dma("sp", xt.full(), x[t0 + tti * 128: t0 + (tti + 1) * 128, :])
            rms_to_hT(xt.full(), tti, 0, tti % 2)

        if qi == 0:
            memset("pool", Rf.full(), 0.0)
            memset("pool", Rb.full(), 0.0)

        if stop <= 1:
            continue
        def proj_fm(key, dst_fn, post):
            wt = wget(key)
            for m in range(4):
                b = next_bank()
                for k in range(8):
                    mm(banks[b][0].full(), wt[:, k, m * 128:(m + 1) * 128], hT[:, k, :], k == 0, k == 7)
                post(m, banks[b][0].full())

        def proj_tm(key, post):
            wt = wget(key)
            for tti in range(4):
                b = next_bank()
                for k in range(8):
                    mm(banks[b][0].full(), hT[:, k, tti * 128:(tti + 1) * 128], wt[:, k, :], k == 0, k == 7)
                post(tti, banks[b][0].full())

        for j in range(2):
            proj_fm(("qa", j), None, lambda m, ps, j=j: evac_copy(U[:, U_QT + 4 * j + m, :], ps, 0.125))
        for j in range(2):
            proj_fm(("ka", j), None, lambda m, ps, j=j: evac_copy(KT[:, 4 * j + m, s0:s0 + QB], ps))
        for j in range(2):
            proj_tm(("va", j), lambda tti, ps, j=j: evac_copy(VC[:, qi * 4 + tti, j * 512:(j + 1) * 512], ps))
        qdec_b = qdec.full().with_ap(lambda ap: ap.unsqueeze(1).broadcast_to([128, 4, 4, 128]))
        def post_qr(m, ps):
            tt("dve", U[:, U_QR + m, :].with_ap(lambda ap: ap.rearrange("p (t n) -> p t n", n=128)),
               ps.with_ap(lambda ap: ap.rearrange("p (t n) -> p t n", n=128)),
               qdec[:, m, :].with_ap(lambda ap: ap.unsqueeze(1).broadcast_to([128, 4, 128])), ALU.mult)
        def post_kr(m, ps):
            tt("dve", U[:, U_KR + m, :].with_ap(lambda ap: ap.rearrange("p (t n) -> p t n", n=128)),
               ps.with_ap(lambda ap: ap.rearrange("p (t n) -> p t n", n=128)),
               kdec[:, m, :].with_ap(lambda ap: ap.unsqueeze(1).broadcast_to([128, 4, 128])), ALU.mult)
        def filler_gen():
            for key, post in ((("qr", 0), post_qr), (("kr", 0), post_kr)):
                wt = wget(key)
                for m in range(4):
                    b = next_bank()
                    for k in range(8):
                        mm(banks[b][0].full(), wt[:, k, m * 128:(m + 1) * 128], hT[:, k, :], k == 0, k == 7)
                    post(m, banks[b][0].full())
                    yield
            for j in range(2):
                wt = wget(("vr", j))
                for tti in range(4):
                    b = next_bank()
                    for k in range(8):
                        mm(banks[b][0].full(), hT[:, k, tti * 128:(tti + 1) * 128], wt[:, k, :], k == 0, k == 7)
                    evac_copy(U[:, U_VR + 2 * tti + j, :], banks[b][0].full())
                    yield
            for j in range(2):
                wt = wget(("gr", j))
                for m in range(4):
                    b = next_bank()
                    for k in range(8):
                        mm(banks[b][0].full(), wt[:, k, m * 128:(m + 1) * 128], hT[:, k, :], k == 0, k == 7)
                    post_gr(m, banks[b][0].full(), j)
                    yield
            for tti in range(4):
                b = next_bank()
                for hh in range(4):
                    tr(banks[b][1][:, hh, :], U[:, U_KR + hh, tti * 128:(tti + 1) * 128])
                tt("dve", U[:, U_KRT + tti, :].with_ap(lambda ap: ap.rearrange("p (h n) -> p h n", n=128)),
                   banks[b][1][:, 0:4, :], cdec.full().with_ap(lambda ap: ap.unsqueeze(2).broadcast_to([128, 4, 128])), ALU.mult)
                yield
        filler = filler_gen()
        def post_gr(m, ps, j):
            th = sqb_d.full()
            act(th, ps, AF.Tanh, scale=0.5)
            stt("dve", U[:, U_GR + 4 * j + m, :], th, 1.0, ps, ALU.add, ALU.mult)
        if stop <= 2:
            for _ in filler:
                pass

        if stop <= 2:
            continue
        for h in range(NH):
            W = SUBW[h]
            slope = SLOPES[h]
            acc = []
            for _ in range(2):
                b = next_bank()
                held.add(b)
                acc.append(b)
            O = [banks[acc[0]][0], banks[acc[1]][0]]
            den = [tmp_half(), tmp_half()]
            nkt = qi * 4 + 4
            jlist = []
            for j in range(nkt):
                k0 = j * 128
                mind = max(0, s0 - (k0 + 127))
                if slope * mind > CUT:
                    continue
                jlist.append(j)
            staged = {}

            def stage(j):
                k0 = j * 128
                r = j - qi * 4
                a_lo = max(0, r) * 128
                Sb = []
                for c in range(2):
                    b = next_bank()
                    S = banks[b][0]
                    mm(S[:, a_lo:QB], KT[64 * c:64 * c + 64, h, k0:k0 + 128], U[64 * c:64 * c + 64, U_QT + h, a_lo:QB], True, True)
                    Sb.append(S)
                pts = []
                for c in range(2):
                    S = Sb[c]
                    pt = PT[cnt["pt"] % 4]
                    cnt["pt"] += 1
                    for sb0 in range(0, QB, W):
                        lo = max(sb0, a_lo)
                        hi = sb0 + W
                        if lo >= hi:
                            continue
                        delta = s0 + sb0 - k0
                        act(pt[:, lo:hi], S[:, lo:hi], AF.Exp, bias=bias_col(h, delta))
                    if r >= 0:
                        dsl = pt[:, a_lo:a_lo + 128]
                        P.op("pool", lambda e, dsl=dsl: e.affine_select(out=dsl.ap, in_=dsl.ap, pattern=[[1, 128]],
                                                                        compare_op=ALU.is_ge, fill=0.0, base=0, channel_multiplier=-1),
                             reads=[dsl], writes=[dsl])
                    pts.append(pt)
                staged[j] = (pts, a_lo)

            def consume(j):
                pts, a_lo = staged.pop(j)
                st = (j == jlist[0])
                last = (j == jlist[-1])
                assert a_lo == 0 or not st
                for c in range(2):
                    mm(O[c][:, a_lo:QB], VC[:, j, h * 128:(h + 1) * 128], pts[c][:, a_lo:QB], st, last)
                for c in range(2):
                    if st:
                        cp("dve", den[c], pts[c].full())
                    else:
                        tt("dve", den[c].cols(a_lo, QB), den[c].cols(a_lo, QB), pts[c][:, a_lo:QB], ALU.add)

            n_it = len(jlist)
            stage(jlist[0])
            run_deferred(1)
            for i in range(n_it):
                if i + 1 < n_it:
                    stage(jlist[i + 1])
                consume(jlist[i])
                next(filler, None)
                if i == min(3, n_it - 1):
                    run_deferred(2)

            def part_a(O=O, den=den, acc=acc):
                for c in range(2):
                    b = next_bank()
                    mm(banks[b][0].full(), ones_f.full(), den[c], True, True)
                    act(den[c], banks[b][0].full(), AF.Ln, scale=2.0 ** -8)
                    act(den[c], den[c], AF.Exp, scale=-1.0, bias=lnk)
                tt("dve", den[0], O[0].full(), den[0], ALU.mult)
                tt("dve", den[1], O[1].full(), den[1], ALU.mult)
                for b in acc:
                    held.discard(b)
                stt("dve", yraw_d.full(), den[1], neg_lam, den[0], ALU.mult, ALU.add)

            def part_b(h=h):
                act(sqb_d.full(), yraw_d.full(), AF.Square)
                b = next_bank()
                mm(banks[b][0].full(), ones.full(), sqb_d.full(), True, True)
                rstd = tmp_half()
                act(rstd, banks[b][0].full(), AF.Ln, scale=1.0 / 128, bias=epsc[:, 0:1])
                act(rstd, rstd, AF.Exp, scale=-0.5)
                stt("dve", U[:, U_YA + h, :], yraw_d.full(), gdcol.full(), rstd, ALU.mult, ALU.mult)
            run_deferred(2)
            deferred.append([part_a, part_b])
            run_deferred(1)

        if stop <= 3:
            continue
        def ret_scores(tti):
            tsl = slice(tti * 128, (tti + 1) * 128)
            b = next_bank()
            for h in range(4):
                mm(banks[b][0][:, h * 128:(h + 1) * 128], U[:, U_KR + h, tsl], U[:, U_QR + h, tsl], True, True)
            sm = Sm[tti % 2]
            tt("dve", sm.full(), banks[b][0].full().with_ap(lambda ap: ap.rearrange("p (h n) -> p h n", n=128)),
               trimask.full().with_ap(lambda ap: ap.unsqueeze(1).broadcast_to([128, 4, 128])), ALU.mult)

        def ret_transposes(tti):
            tsl = slice(tti * 128, (tti + 1) * 128)
            yt = yrtok[tti % 2]
            b = next_bank()
            for k in range(8):
                tr(banks[b][1][:, k, :], yt[:, k * 128:(k + 1) * 128])
            tt("dve", U[:, U_YR:U_YR + 8, tsl], banks[b][1].full(), U[:, U_GR:U_GR + 8, tsl], ALU.mult)

        for _ in filler:
            pass
        ret_scores(0)
        for tti in range(4):
            tsl = slice(tti * 128, (tti + 1) * 128)
            rb_ = [next_bank(), next_bank()]
            for h in range(4):
                o = banks[rb_[h // 2]][0][:, (h % 2) * 256:(h % 2 + 1) * 256]
                vr = U[:, U_VR + 2 * tti + h // 2, (h % 2) * 256:(h % 2 + 1) * 256]
                mm(o, U[:, U_KRT + tti, h * 128:(h + 1) * 128], vr, True, True)
            if tti < 3:
                ret_scores(tti + 1)
            if tti == 0:
                run_deferred(1)
            if tti == 1:
                run_deferred(2)
            sm = Sm[tti % 2]
            ob = [next_bank(), next_bank()]
            for h in range(4):
                o = banks[ob[h // 2]][0][:, (h % 2) * 256:(h % 2 + 1) * 256]
                vr = U[:, U_VR + 2 * tti + h // 2, (h % 2) * 256:(h % 2 + 1) * 256]
                mm(o, sm[:, h, :], vr, True, False)
                mm(o, U[:, U_QR + h, tsl], Rb[:, h, :], False, True)
            if tti > 0:
                ret_transposes(tti - 1)
            for h in range(4):
                o = banks[rb_[h // 2]][0][:, (h % 2) * 256:(h % 2 + 1) * 256]
                stt("dve", Rf[:, h, :], Rf[:, h, :], GAM[h] ** 128, o, ALU.mult, ALU.add)
            cp("dve", Rb.full(), Rf.full())
            yt = yrtok[tti % 2]
            sc = 4 * (tti % 2)
            junk = hb[tti % 2]
            for h in range(4):
                o = banks[ob[h // 2]][0][:, (h % 2) * 256:(h % 2 + 1) * 256]
                act(junk[:, h * 256:(h + 1) * 256], o, AF.Square, accum=stat[:, sc + h:sc + h + 1])
            rsqrt_cols(stat[:, sc + 8:sc + 12], stat[:, sc:sc + 4], 4, 1.0 / 256)
            for h in range(4):
                o = banks[ob[h // 2]][0][:, (h % 2) * 256:(h % 2 + 1) * 256]
                stt("dve", yt[:, h * 256:(h + 1) * 256], o, stat[:, sc + 8 + h:sc + 9 + h], gret_b[:, h * 256:(h + 1) * 256], ALU.mult, ALU.mult)
        ret_transposes(3)

        if stop <= 4:
            continue
        for tti in range(4):
            P.dma("sp", xres[:, tti, :], x[t0 + tti * 128: t0 + (tti + 1) * 128, :])

        for j in range(2):
            wt = wget(("ga", j))
            for m in range(4):
                b = next_bank()
                for k in range(8):
                    mm(banks[b][0].full(), wt[:, k, m * 128:(m + 1) * 128], hT[:, k, :], k == 0, k == 7)
                act(U[:, U_TA + m, :], banks[b][0].full(), AF.Tanh, scale=0.5)
            wt = wget(("wd", j))
            for m in range(4):
                b = next_bank()
                for k in range(8):
                    mm(banks[b][0].full(), wt[:, k, m * 128:(m + 1) * 128], U[:, U_YA + k, :], k == 0, k == 7)
                stt("dve", U[:, U_MIX + 4 * j + m, :], U[:, U_TA + m, :], 1.0, banks[b][0].full(), ALU.add, ALU.mult)
            wt = wget(("gb", j))
            for m in range(4):
                b = next_bank()
                for k in range(8):
                    mm(banks[b][0].full(), wt[:, k, m * 128:(m + 1) * 128], hT[:, k, :], k == 0, k == 7)
                act(U[:, U_TA + m, :], banks[b][0].full(), AF.Tanh, scale=0.5)
            wt = wget(("wr", j))
            for m in range(4):
                b = next_bank()
                for k in range(8):
                    mm(banks[b][0].full(), wt[:, k, m * 128:(m + 1) * 128], U[:, U_YR + k, :], k == 0, k == 7)
                th = tmp_half()
                stt("dve", th, U[:, U_TA + m, :], 1.0, banks[b][0].full(), ALU.add, ALU.mult)
                tt("dve", U[:, U_MIX + 4 * j + m, :], U[:, U_MIX + 4 * j + m, :], th, ALU.add)

        if stop <= 5:
            continue
        wts = [wget(("wo", 0)), wget(("wo", 1), ahead=0)]
        for tti in range(4):
            for n in range(2):
                wt = wts[n]
                b = next_bank()
                for k in range(8):
                    mm(banks[b][0].full(), U[:, U_MIX + k, tti * 128:(tti + 1) * 128], wt[:, k, :], k == 0, k == 7)
                xs = xres[:, tti, n * 512:(n + 1) * 512]
                stt("dve", xs, banks[b][0].full(), 0.5, xs, ALU.mult, ALU.add)
            if tti > 1:
                rms_to_hT(xres[:, tti - 2, :], tti - 2, 1, (tti - 2) % 2)
        rms_to_hT(xres[:, 2, :], 2, 1, 0)
        rms_to_hT(xres[:, 3, :], 3, 1, 1)

        if stop <= 7:
            continue

        if stop <= 7:
            continue
        for j in range(8):
            wt = wget(("f1", j))
            for m in range(4):
                b = next_bank()
                for k in range(8):
                    mm(banks[b][0].full(), wt[:, k, m * 128:(m + 1) * 128], hT[:, k, :], k == 0, k == 7)
                th = tmp_half()
                act(th, banks[b][0].full(), AF.Square)
                stt("dve", U[:, 4 * j + m, :], banks[b][0].full(), 0.0, th, ALU.is_gt, ALU.mult)

        if stop <= 8:
            continue
        for n in range(2):
            accb = []
            for _ in range(4):
                b = next_bank()
                held.add(b)
                accb.append(b)
            for g in range(4):
                wt = wget(("f2", n * 4 + g))
                for tti in range(4):
                    for k in range(8):
                        mm(banks[accb[tti]][0].full(), U[:, g * 8 + k, tti * 128:(tti + 1) * 128], wt[:, k, :],
                           g == 0 and k == 0, g == 3 and k == 7)
            for tti in range(4):
                xs = xres[:, tti, n * 512:(n + 1) * 512]
                tt("dve", xs, banks[accb[tti]][0].full(), xs, ALU.add)
                held.discard(accb[tti])

        if stop <= 9:
            continue
        for tti in range(4):
            rms_to_hT(xres[:, tti, :], tti, 2, tti % 2)
        wt = wget(("ple", 0))
        for tti in range(4):
            pi = pinb[tti % 2]
            P.dma("sp", pi.full(), pin[t0 + tti * 128: t0 + (tti + 1) * 128, :])
            cp("pool", pb16.full(), pi.full())
            b = next_bank()
            for kc in range(2):
                tr(banks[b][1][:, kc, :], pb16[:, kc * 128:(kc + 1) * 128])
            cp("dve", ppT[:, :, tti * 128:(tti + 1) * 128], banks[b][1][:, 0:2, :])
            for n in range(2):
                b = next_bank()
                for kc in range(2):
                    mm(banks[b][0].full(), ppT[:, kc, tti * 128:(tti + 1) * 128], wt[:, 2 * n + kc, :], kc == 0, kc == 1)
                evac_copy(U[:, U_YA + 2 * tti + n, :], banks[b][0].full())
        for n in range(2):
            wt = wget(("pg", n))
            for tti in range(4):
                b = next_bank()
                for k in range(8):
                    mm(banks[b][0].full(), hT[:, k, tti * 128:(tti + 1) * 128], wt[:, k, :], k == 0, k == 7)
                th = tmp_half()
                act(th, banks[b][0].full(), AF.Tanh, scale=0.5)
                stt("dve", th, th, 1.0, U[:, U_YA + 2 * tti + n, :], ALU.add, ALU.mult)
                xs = xres[:, tti, n * 512:(n + 1) * 512]
                stt("dve", xs, th, 0.5, xs, ALU.mult, ALU.add)
        for tti in range(4):
            sc = tti % 2
            junk = hb[sc]
            act(junk.full(), xres[:, tti, :], AF.Square, accum=stat[:, sc:sc + 1])
            rsqrt_cols(stat[:, sc + 8:sc + 9], stat[:, sc:sc + 1], 1, 1.0 / D)
            ot = tmp_full()
            stt("dve", ot.full(), xres[:, tti, :], stat[:, sc + 8:sc + 9], gfin_b.full(), ALU.mult, ALU.mult)
            P.dma("pool", out[t0 + tti * 128: t0 + (tti + 1) * 128, :], ot.full())

    P.barrier("pool", [out.full()])
    P.barrier("sp", [out.full()])
    P.emit()
    P.close()
    return nc, P


_CACHE = {}


def kernel(**inputs):
    n = 8
    xs = np.ascontiguousarray(np.asarray(inputs["x"], dtype=np.float32)).reshape(n, TOK, D)
    ps = np.ascontiguousarray(np.asarray(inputs["p"], dtype=np.float32)).reshape(n, TOK, PLE)
    shared = {}
    for k in ("g_mix", "g_mlp", "g_ple", "w_in", "w_branch_diff", "w_branch_ret", "w_out", "w_ff1", "w_ff2",
              "w_ple_gate", "w_ple", "lam_q1", "lam_k1", "lam_q2", "lam_k2", "g_diff_sub"):
        a = np.asarray(inputs[k], dtype=np.float32)
        shared[k] = np.ascontiguousarray(a.reshape(a.shape[1:]))
    shared["g_ret_sub"] = np.ascontiguousarray(np.asarray(inputs["g_ret_sub"], dtype=np.float32).reshape(1024))
    shared["g_final"] = np.ascontiguousarray(np.asarray(inputs["g_final"], dtype=np.float32))
    if "nc" not in _CACHE:
        _CACHE["nc"] = build_program()[0]
    nc = _CACHE["nc"]
    in_maps = [dict(shared, x=xs[i], p=ps[i]) for i in range(n)]
    res = run_bass_kernel_spmd(nc, in_maps, core_ids=list(range(n)))
    outs = [np.asarray(res.results[i]["out"], dtype=np.float32).reshape(NSEQ, SEQ, D) for i in range(n)]
    return np.concatenate(outs, axis=0)
```
